# Optimizing a Trainium2 kernel written in Bass

```python
import jax, jax.numpy as jnp
from jax import lax
import numpy as np

D_MODEL = 1024
BATCH = 4
SEQ = 8192
DEPTH = 2

GRID_W = 64
CTX_LEN = 256
D_MIX = D_MODEL
N_GROUPS = 4
GROUP_W = D_MIX // N_GROUPS
NA_HEADS = 4
NA_HEAD_DIM = GROUP_W // NA_HEADS
NA_WIN_R_MAX = 8
NA_WIN_C = 16
POOL_WINDOWS = (2, 4, 8, 16)
POOL_CH = GROUP_W // len(POOL_WINDOWS)
FNO_GROUPS = 4
FNO_CH = GROUP_W // FNO_GROUPS
MLA_HEADS = 4
MLA_NOPE = 64
MLA_ROPE = 32
MLA_V = GROUP_W // MLA_HEADS
MLA_Q_RANK = 256
MLA_KV_RANK = 128
ROPE_BASE = 10000.0
Q_BLOCK = 128
PROJ_SIZES = (GROUP_W, GROUP_W, GROUP_W, GROUP_W, GROUP_W, MLA_Q_RANK, MLA_KV_RANK, MLA_ROPE)
D_IN = 5 * GROUP_W + MLA_Q_RANK + MLA_KV_RANK + MLA_ROPE
N_EXPERTS = 16
EC_CAPACITY_FACTOR = 2
D_EXPERT = 1024
EPS = 1e-6

kernel_name = 'hybrid_diffusion_parallel_head_groups'


def _rmsnorm(x, g):
    xf = x.astype(jnp.float32)
    y = xf * lax.rsqrt(jnp.mean(xf * xf, axis=-1, keepdims=True) + EPS)
    return (y * g.astype(jnp.float32)).astype(x.dtype)


def _modulate(x, g, shift, scale):
    return _rmsnorm(x, g) * (1 + scale) + shift


def _adaln(cvec, ada_w, ada_b):
    m = jax.nn.silu(cvec) @ ada_w + ada_b
    return jnp.split(m, 6, axis=-1)


def _rotate(v, ang):
    f = ang.shape[-1]
    cos = jnp.cos(ang)[:, None, :].astype(v.dtype)
    sin = jnp.sin(ang)[:, None, :].astype(v.dtype)
    v1, v2 = v[..., :f], v[..., f:]
    return jnp.concatenate([v1 * cos - v2 * sin, v1 * sin + v2 * cos], axis=-1)


def _rope_2d(v):
    n = v.shape[1]
    t = jnp.arange(n)
    rows = (t // GRID_W).astype(jnp.float32)
    cols = (t % GRID_W).astype(jnp.float32)
    half = v.shape[-1] // 2
    nf = half // 2
    inv = ROPE_BASE ** (-jnp.arange(nf, dtype=jnp.float32) / nf)
    return jnp.concatenate([_rotate(v[..., :half], rows[:, None] * inv),
                            _rotate(v[..., half:], cols[:, None] * inv)], axis=-1)


def _heads(t):
    return t.reshape(t.shape[0], t.shape[1], NA_HEADS, NA_HEAD_DIM)


def _neighbourhood_attn(q, k, v, kc, vc, bias_tab):
    B, N, H, dh = q.shape
    rows = N // GRID_W
    win_r = min(NA_WIN_R_MAX, rows)
    scale = dh ** -0.5
    qg = q.reshape(B, rows, GRID_W, H, dh)
    kg = k.reshape(B, rows, GRID_W, H, dh)
    vg = v.reshape(B, rows, GRID_W, H, dh)
    row_start = jnp.clip(jnp.arange(rows) - win_r // 2, 0, rows - win_r)
    col_start = jnp.clip(jnp.arange(GRID_W) - NA_WIN_C // 2, 0, GRID_W - NA_WIN_C)
    col_idx = col_start[:, None] + jnp.arange(NA_WIN_C)
    dc = col_idx - jnp.arange(GRID_W)[:, None] + (NA_WIN_C - 1)
    nw = win_r * NA_WIN_C

    def row_block(args):
        q_row, qy, rs = args
        k_rows = lax.dynamic_slice_in_dim(kg, rs, win_r, axis=1)
        v_rows = lax.dynamic_slice_in_dim(vg, rs, win_r, axis=1)
        k_win = k_rows[:, :, col_idx]
        v_win = v_rows[:, :, col_idx]
        dr = rs + jnp.arange(win_r) - qy + (NA_WIN_R_MAX - 1)
        bias = bias_tab[:, dr][:, :, dc].transpose(0, 2, 1, 3)
        s_win = (jnp.einsum('bqhd,brqjhd->bhqrj', q_row, k_win).astype(jnp.float32) * scale
                 + bias[None].astype(jnp.float32)).reshape(B, H, GRID_W, nw)
        s_ctx = jnp.einsum('bqhd,blhd->bhql', q_row, kc).astype(jnp.float32) * scale
        p = jax.nn.softmax(jnp.concatenate([s_win, s_ctx], axis=-1), axis=-1)
        p_win = p[..., :nw].reshape(B, H, GRID_W, win_r, NA_WIN_C).astype(v.dtype)
        p_ctx = p[..., nw:].astype(v.dtype)
        return (jnp.einsum('bhqrj,brqjhd->bqhd', p_win, v_win)
                + jnp.einsum('bhql,blhd->bqhd', p_ctx, vc))

    out = lax.map(row_block, (qg.transpose(1, 0, 2, 3, 4), jnp.arange(rows), row_start))
    return out.transpose(1, 0, 2, 3, 4).reshape(B, N, H * dh)


def _block_attn(q, k, v, kc, vc):
    B, N, H, d = q.shape
    dv = v.shape[-1]
    scale = d ** -0.5
    nb = N // Q_BLOCK
    qb = q.reshape(B, nb, Q_BLOCK, H, d).transpose(1, 0, 2, 3, 4)

    def body(qblk):
        s_lat = jnp.einsum('bqhd,bkhd->bhqk', qblk, k).astype(jnp.float32) * scale
        s_ctx = jnp.einsum('bqhd,blhd->bhql', qblk, kc).astype(jnp.float32) * scale
        p = jax.nn.softmax(jnp.concatenate([s_lat, s_ctx], axis=-1), axis=-1)
        p_lat = p[..., :N].astype(v.dtype)
        p_ctx = p[..., N:].astype(v.dtype)
        return (jnp.einsum('bhqk,bkhd->bqhd', p_lat, v)
                + jnp.einsum('bhql,blhd->bqhd', p_ctx, vc))

    out = lax.map(body, qb)
    return out.transpose(1, 0, 2, 3, 4).reshape(B, N, H * dv)


def _dense_attn(q, k, v):
    B, L, H, d = q.shape
    s = jnp.einsum('bqhd,bkhd->bhqk', q, k).astype(jnp.float32) * (d ** -0.5)
    p = jax.nn.softmax(s, axis=-1).astype(v.dtype)
    return jnp.einsum('bhqk,bkhd->bqhd', p, v).reshape(B, L, -1)


def _pool_mix(u, pool_w, pool_scale):
    B, N, _ = u.shape
    uf = u.astype(jnp.float32)
    cs = jnp.concatenate([jnp.zeros((B, 1, GROUP_W), jnp.float32), jnp.cumsum(uf, axis=1)], axis=1)
    t = jnp.arange(N)
    outs = []
    for g, w in enumerate(POOL_WINDOWS):
        lo = jnp.clip(t - w // 2, 0, N)
        hi = jnp.clip(t + w - w // 2, 0, N)
        ch = slice(g * POOL_CH, (g + 1) * POOL_CH)
        mean = (cs[:, hi, ch] - cs[:, lo, ch]) / (hi - lo).astype(jnp.float32)[None, :, None]
        outs.append(mean - uf[:, :, ch])
    pooled = jnp.stack(outs, axis=2).astype(u.dtype)
    y = jnp.einsum('bngc,gcd->bngd', pooled, pool_w).reshape(B, N, GROUP_W)
    return y * pool_scale


def _fourier_mix(u, fno_w):
    B, N, _ = u.shape
    ug = u.astype(jnp.float32).reshape(B, N, FNO_GROUPS, FNO_CH)
    f = jnp.fft.fft2(ug, axes=(1, 3), norm='ortho').real
    return f.reshape(B, N, GROUP_W).astype(u.dtype) @ fno_w


def _mla_q(q_c, q_norm, w_uq, positioned):
    B, N, _ = q_c.shape
    q = (_rmsnorm(q_c, q_norm) @ w_uq).reshape(B, N, MLA_HEADS, MLA_NOPE + MLA_ROPE)
    if positioned:
        q = jnp.concatenate([q[..., :MLA_NOPE], _rope_2d(q[..., MLA_NOPE:])], axis=-1)
    return q


def _mla_kv(kv_c, k_r, kv_norm, w_uk, w_uv, positioned):
    B, N, _ = kv_c.shape
    ckv = _rmsnorm(kv_c, kv_norm)
    k_nope = (ckv @ w_uk).reshape(B, N, MLA_HEADS, MLA_NOPE)
    v = (ckv @ w_uv).reshape(B, N, MLA_HEADS, MLA_V)
    k_rope = k_r[:, :, None, :]
    if positioned:
        k_rope = _rope_2d(k_rope)
    k = jnp.concatenate([k_nope, jnp.broadcast_to(k_rope, (B, N, MLA_HEADS, MLA_ROPE))], axis=-1)
    return k, v


def _merge(outs, grp_norm, w_out):
    cat = jnp.concatenate([_rmsnorm(o, grp_norm[g]) for g, o in enumerate(outs)], axis=-1)
    return cat @ w_out


def _mixer(hl, hc, w_in, na_bias, pool_w, pool_scale, fno_w, mla_q_norm, mla_w_uq, mla_kv_norm,
           mla_w_uk, mla_w_uv, grp_norm, w_out, with_ctx):
    cuts = np.cumsum(PROJ_SIZES)[:-1].tolist()
    qa_l, ka_l, va_l, pl_l, fl_l, qc_l, kvc_l, kr_l = jnp.split(hl @ w_in, cuts, axis=-1)
    qa_c, ka_c, va_c, pl_c, fl_c, qc_c, kvc_c, kr_c = jnp.split(hc @ w_in, cuts, axis=-1)
    kD_c, vD_c = _mla_kv(kvc_c, kr_c, mla_kv_norm, mla_w_uk, mla_w_uv, False)
    kD_l, vD_l = _mla_kv(kvc_l, kr_l, mla_kv_norm, mla_w_uk, mla_w_uv, True)
    qD_l = _mla_q(qc_l, mla_q_norm, mla_w_uq, True)
    a_l = _neighbourhood_attn(_heads(qa_l), _heads(ka_l), _heads(va_l), _heads(ka_c), _heads(va_c), na_bias)
    b_l = _pool_mix(pl_l, pool_w, pool_scale)
    c_l = _fourier_mix(fl_l, fno_w)
    d_l = _block_attn(qD_l, kD_l, vD_l, kD_c, vD_c)
    out_l = _merge((a_l, b_l, c_l, d_l), grp_norm, w_out)
    if not with_ctx:
        return out_l, None
    qD_c = _mla_q(qc_c, mla_q_norm, mla_w_uq, False)
    a_c = _dense_attn(_heads(qa_c), _heads(ka_c), _heads(va_c))
    b_c = _pool_mix(pl_c, pool_w, pool_scale)
    c_c = _fourier_mix(fl_c, fno_w)
    d_c = _dense_attn(qD_c, kD_c, vD_c)
    out_c = _merge((a_c, b_c, c_c, d_c), grp_norm, w_out)
    return out_l, out_c


def _ec_moe(h, router_w, w_gate, w_up, w_down):
    B, N, D = h.shape
    aff = jax.nn.softmax((h @ router_w).astype(jnp.float32), axis=-1)
    cap = max(1, EC_CAPACITY_FACTOR * N // N_EXPERTS)
    gates, idx = lax.top_k(jnp.swapaxes(aff, 1, 2), cap)
    bidx = jnp.arange(B)[:, None, None]
    xs = h[bidx, idx]
    hid = (jax.nn.silu(jnp.einsum('becd,edf->becf', xs, w_gate))
           * jnp.einsum('becd,edf->becf', xs, w_up))
    y = jnp.einsum('becf,efd->becd', hid, w_down) * gates[..., None].astype(h.dtype)
    return jnp.zeros_like(h).at[bidx, idx].add(y)


def setup_inputs(seed: int = 0) -> dict:
    key = jax.random.key(seed)
    ks = jax.random.split(key, 26)
    nrm = jax.random.normal
    f32 = jnp.float32
    return {
        'x': nrm(ks[0], (BATCH, SEQ, D_MODEL), f32),
        'c': nrm(ks[1], (BATCH, D_MODEL), f32),
        'ctx': nrm(ks[2], (BATCH, CTX_LEN, D_MODEL), f32),
        'c_ctx': nrm(ks[3], (D_MODEL,), f32),
        'ada_w': nrm(ks[4], (DEPTH, D_MODEL, 6 * D_MODEL), f32) * (0.5 * D_MODEL ** -0.5),
        'ada_b': nrm(ks[5], (DEPTH, 6 * D_MODEL), f32) * 0.02,
        'norm1_g': 1.0 + 0.05 * nrm(ks[6], (DEPTH, D_MODEL), f32),
        'norm2_g': 1.0 + 0.05 * nrm(ks[7], (DEPTH, D_MODEL), f32),
        'w_in': nrm(ks[8], (DEPTH, D_MODEL, D_IN), f32) * D_MODEL ** -0.5,
        'na_bias': nrm(ks[9], (DEPTH, NA_HEADS, 2 * NA_WIN_R_MAX - 1, 2 * NA_WIN_C - 1), f32) * 0.1,
        'pool_w': nrm(ks[10], (DEPTH, len(POOL_WINDOWS), POOL_CH, POOL_CH), f32) * POOL_CH ** -0.5,
        'pool_scale': 1.0 + 0.05 * nrm(ks[11], (DEPTH, GROUP_W), f32),
        'fno_w': nrm(ks[12], (DEPTH, GROUP_W, GROUP_W), f32) * GROUP_W ** -0.5,
        'mla_q_norm': 1.0 + 0.05 * nrm(ks[13], (DEPTH, MLA_Q_RANK), f32),
        'mla_w_uq': nrm(ks[14], (DEPTH, MLA_Q_RANK, MLA_HEADS * (MLA_NOPE + MLA_ROPE)), f32) * MLA_Q_RANK ** -0.5,
        'mla_kv_norm': 1.0 + 0.05 * nrm(ks[15], (DEPTH, MLA_KV_RANK), f32),
        'mla_w_uk': nrm(ks[16], (DEPTH, MLA_KV_RANK, MLA_HEADS * MLA_NOPE), f32) * MLA_KV_RANK ** -0.5,
        'mla_w_uv': nrm(ks[17], (DEPTH, MLA_KV_RANK, MLA_HEADS * MLA_V), f32) * MLA_KV_RANK ** -0.5,
        'grp_norm': 1.0 + 0.05 * nrm(ks[18], (DEPTH, N_GROUPS, GROUP_W), f32),
        'w_out': nrm(ks[19], (DEPTH, D_MIX, D_MODEL), f32) * D_MIX ** -0.5,
        'router_w': nrm(ks[20], (DEPTH, D_MODEL, N_EXPERTS), f32) * D_MODEL ** -0.5,
        'exp_w_gate': nrm(ks[21], (DEPTH, N_EXPERTS, D_MODEL, D_EXPERT), f32) * D_MODEL ** -0.5,
        'exp_w_up': nrm(ks[22], (DEPTH, N_EXPERTS, D_MODEL, D_EXPERT), f32) * D_MODEL ** -0.5,
        'exp_w_down': nrm(ks[23], (DEPTH, N_EXPERTS, D_EXPERT, D_MODEL), f32) * D_EXPERT ** -0.5,
        'final_norm': 1.0 + 0.05 * nrm(ks[24], (D_MODEL,), f32),
    }


def reference(x, c, ctx, c_ctx, ada_w, ada_b, norm1_g, norm2_g, w_in, na_bias, pool_w, pool_scale, fno_w,
              mla_q_norm, mla_w_uq, mla_kv_norm, mla_w_uk, mla_w_uv, grp_norm, w_out, router_w,
              exp_w_gate, exp_w_up, exp_w_down, final_norm):
    xl, xc = x, ctx
    for i in range(DEPTH):
        last = i == DEPTH - 1
        sh1, sc1, gt1, sh2, sc2, gt2 = [m[:, None, :] for m in _adaln(c, ada_w[i], ada_b[i])]
        csh1, csc1, cgt1, csh2, csc2, cgt2 = _adaln(c_ctx, ada_w[i], ada_b[i])
        hl = _modulate(xl, norm1_g[i], sh1, sc1)
        hc = _modulate(xc, norm1_g[i], csh1, csc1)
        mix_l, mix_c = _mixer(hl, hc, w_in[i], na_bias[i], pool_w[i], pool_scale[i], fno_w[i],
                              mla_q_norm[i], mla_w_uq[i], mla_kv_norm[i], mla_w_uk[i], mla_w_uv[i],
                              grp_norm[i], w_out[i], not last)
        xl = xl + gt1 * mix_l
        xl = xl + gt2 * _ec_moe(_modulate(xl, norm2_g[i], sh2, sc2), router_w[i],
                                exp_w_gate[i], exp_w_up[i], exp_w_down[i])
        if not last:
            xc = xc + cgt1 * mix_c
            xc = xc + cgt2 * _ec_moe(_modulate(xc, norm2_g[i], csh2, csc2), router_w[i],
                                     exp_w_gate[i], exp_w_up[i], exp_w_down[i])
    return _rmsnorm(xl, final_norm)
```

```python
import math
from contextlib import ExitStack
import numpy as np
import concourse.bass as bass
import concourse.mybir as mybir
from concourse.bass_utils import run_bass_kernel_spmd

F32 = mybir.dt.float32
BF16 = mybir.dt.bfloat16
I32 = mybir.dt.int32
AF = mybir.ActivationFunctionType
ALU = mybir.AluOpType
AX = mybir.AxisListType

NL = 8192
NC_ = 256
TT = NL + NC_
D = 1024
NTILE = TT // 128
EPS = 1e-6
NEXP = 16
NEL = 8
NXCH = 17
CAP_L = 1024
CAP_C = 32
RW = 1028
BIG = 1.0e6
SAME_ENGINE_NOWAIT = ("pe",)


class T:
    __slots__ = ("h", "writes", "reads", "name", "loose")

    def __init__(self, h, name="", loose=False):
        self.h = h
        self.writes = {}
        self.reads = {}
        self.name = name
        self.loose = loose

    def __getitem__(self, k):
        return V(self, self.h[k])


class V:
    __slots__ = ("t", "ap")

    def __init__(self, t, ap):
        self.t = t
        self.ap = ap

    def __getitem__(self, k):
        return V(self.t, self.ap[k])

    def rearrange(self, s, **kw):
        return V(self.t, self.ap.rearrange(s, **kw))

    def bitcast(self, dt):
        return V(self.t, self.ap.bitcast(dt))

    def to_broadcast(self, shape):
        return V(self.t, self.ap.to_broadcast(shape))

    def unsqueeze(self, a):
        return V(self.t, self.ap.unsqueeze(a))

    def partition_broadcast(self, n):
        return V(self.t, self.ap.partition_broadcast(n))


def _ap(x):
    return x.ap if isinstance(x, V) else x


class FW:
    ENGS = ("pe", "act", "dve", "pool", "sp")

    def __init__(self, nc, ndma=8):
        self.nc = nc
        self.eng = {"pe": nc.tensor, "act": nc.scalar, "dve": nc.vector, "pool": nc.gpsimd, "sp": nc.sync}
        self.sem = {}
        self.cnt = {}
        for e in ("pe", "act", "dve", "pool"):
            self.sem[e] = nc.alloc_semaphore("S_" + e)
            self.cnt[e] = 0
        self.dring = {}
        for q in ("sp", "pool"):
            ring = []
            for i in range(ndma):
                key = "D_%s_%d" % (q, i)
                self.sem[key] = nc.alloc_semaphore(key)
                self.cnt[key] = 0
                ring.append(key)
            self.dring[q] = [ring, 0]
        self.sem["cc"] = nc.alloc_semaphore("S_cc")
        self.cnt["cc"] = 0
        self.known = {e: {} for e in self.ENGS}
        self.ninstr = 0
        self._ev = 0

    def _wait(self, e, k, v):
        if e == k and e in SAME_ENGINE_NOWAIT:
            return
        kn = self.known[e]
        if kn.get(k, 0) >= v:
            return
        kn[k] = v
        self.eng[e].wait_ge(self.sem[k], v)
        self.ninstr += 1

    def _deps(self, e, ins, outs):
        for x in ins:
            if isinstance(x, V):
                for k, v in x.t.writes.items():
                    self._wait(e, k, v)
        for x in outs:
            t = x.t
            if t.loose:
                continue
            for k, v in t.writes.items():
                self._wait(e, k, v)
            for k, v in t.reads.items():
                self._wait(e, k, v)

    def _done(self, k, v, ins, outs):
        for x in ins:
            if isinstance(x, V):
                r = x.t.reads
                if r.get(k, 0) < v:
                    r[k] = v
        for x in outs:
            t = x.t
            if t.loose:
                if t.writes.get(k, 0) < v:
                    t.writes[k] = v
            else:
                t.writes = {k: v}
                t.reads = {}

    def op(self, e, fn, outs, ins):
        self._deps(e, ins, outs)
        inst = fn(self.eng[e])
        self.cnt[e] += 1
        inst.then_inc(self.sem[e], 1)
        self.ninstr += 1
        self._done(e, self.cnt[e], ins, outs)
        return inst

    def dma(self, q, out, in_, fn=None, deps=(), **kw):
        ring, pos = self.dring[q]
        key = ring[pos % len(ring)]
        self.dring[q][1] = pos + 1
        if self.cnt[key] > 0:
            self._wait(q, key, self.cnt[key])
        self._deps(q, [in_] + list(deps), [out])
        if fn is None:
            inst = self.eng[q].dma_start(out=_ap(out), in_=_ap(in_), **kw)
        else:
            inst = fn(self.eng[q])
        self.cnt[key] += 16
        inst.then_inc(self.sem[key], 16)
        self.ninstr += 1
        self._done(key, self.cnt[key], [in_] + list(deps), [out])
        return inst

    def collective(self, out, in_, fn):
        q = "pool"
        if self.cnt["cc"] > 0:
            self._wait(q, "cc", self.cnt["cc"])
        self._deps(q, [in_], [out])
        inst = fn(self.eng[q])
        self.cnt["cc"] += 1
        inst.then_inc(self.sem["cc"], 1)
        self.ninstr += 1
        self._done("cc", self.cnt["cc"], [in_], [out])
        return inst

    def barrier(self):
        toks = [(k, v) for k, v in self.cnt.items() if v > 0]
        for e in self.ENGS:
            for k, v in toks:
                self._wait(e, k, v)

    def matmul(self, out, lhsT, rhs, start=True, stop=True):
        return self.op("pe", lambda E: E.matmul(_ap(out), _ap(lhsT), _ap(rhs), start=start, stop=stop),
                       [out], [lhsT, rhs])

    def transpose(self, out, in_, ident):
        return self.op("pe", lambda E: E.transpose(_ap(out), _ap(in_), _ap(ident)), [out], [in_, ident])

    def act(self, out, in_, func, bias=None, scale=None, accum_out=None):
        kw = {}
        ins = [in_]
        outs = [out]
        if bias is not None:
            kw["bias"] = _ap(bias)
            ins.append(bias)
        if scale is not None:
            kw["scale"] = _ap(scale)
            ins.append(scale)
        if accum_out is not None:
            kw["accum_out"] = _ap(accum_out)
            outs.append(accum_out)
        return self.op("act", lambda E: E.activation(_ap(out), _ap(in_), func, **kw), outs, ins)

    def tt(self, e, out, in0, in1, op):
        return self.op(e, lambda E: E.tensor_tensor(_ap(out), _ap(in0), _ap(in1), op), [out], [in0, in1])

    def ts(self, e, out, in0, s1, s2, op0, op1=None, accum_out=None):
        kw = {}
        outs = [out]
        if op1 is not None:
            kw["op1"] = op1
        if accum_out is not None:
            kw["accum_out"] = _ap(accum_out)
            outs.append(accum_out)
        return self.op(e, lambda E: E.tensor_scalar(_ap(out), _ap(in0), _ap(s1), _ap(s2), op0, **kw),
                       outs, [in0, s1, s2])

    def stt(self, e, out, in0, scalar, in1, op0, op1):
        return self.op(e, lambda E: E.scalar_tensor_tensor(_ap(out), _ap(in0), _ap(scalar), _ap(in1), op0, op1),
                       [out], [in0, scalar, in1])

    def copy(self, e, out, in_):
        if e == "act":
            return self.op(e, lambda E: E.copy(_ap(out), _ap(in_)), [out], [in_])
        return self.op(e, lambda E: E.tensor_copy(_ap(out), _ap(in_)), [out], [in_])

    def evac(self, out, in_):
        self._ev += 1
        return self.copy("act" if self._ev % 2 else "dve", out, in_)

    def memset(self, e, out, val):
        return self.op(e, lambda E: E.memset(_ap(out), val), [out], [])

    def recip(self, out, in_):
        return self.op("dve", lambda E: E.reciprocal(_ap(out), _ap(in_)), [out], [in_])

    def reduce(self, e, out, in_, axis, op):
        return self.op(e, lambda E: E.tensor_reduce(_ap(out), _ap(in_), axis, op), [out], [in_])


def _consts():
    c = {}
    c["ident"] = np.eye(128, dtype=np.float32)
    t = np.arange(NL)
    rows = (t // 64).astype(np.float64)
    cols = (t % 64).astype(np.float64)
    inv = 10000.0 ** (-np.arange(8, dtype=np.float64) / 8)
    rc = np.zeros((32, NL), np.float64)
    rs = np.zeros((32, NL), np.float64)
    for i in range(8):
        for base, pos in ((0, rows), (16, cols)):
            ang = pos * inv[i]
            rc[base + i] = np.cos(ang); rs[base + i] = -np.sin(ang)
            rc[base + 8 + i] = np.cos(ang); rs[base + 8 + i] = np.sin(ang)
    c["ropeC"] = rc.astype(np.float32)
    c["ropeS"] = rs.astype(np.float32)
    k = np.arange(128, dtype=np.float64)
    a = 2 * np.pi * np.outer(k, k) / 128
    c["c128"] = np.cos(a).astype(np.float32)
    c["s128"] = np.sin(a).astype(np.float32)
    k64 = np.arange(64, dtype=np.float64)
    a64 = 2 * np.pi * np.outer(k64, k64) / 64
    bdc = np.zeros((2, 128, 128), np.float32)
    bds = np.zeros((2, 128, 128), np.float32)
    for ch in range(2):
        for g in range(2):
            bdc[ch, g * 64:(g + 1) * 64, g * 64:(g + 1) * 64] = np.cos(a64)
            bds[ch, g * 64:(g + 1) * 64, g * 64:(g + 1) * 64] = np.sin(a64)
    c["bdc64"] = bdc
    c["bds64"] = bds
    n2 = np.arange(64, dtype=np.float64)[:, None, None]
    k1 = np.arange(128, dtype=np.float64)[None, :, None]
    k2 = np.arange(64, dtype=np.float64)[None, None, :]
    ang = 2 * np.pi * n2 * (k1 + 128 * k2) / 8192
    c["c2"] = np.cos(ang).astype(np.float32)
    c["s2"] = np.sin(ang).astype(np.float32)
    k256 = np.arange(256, dtype=np.float64)
    a256 = 2 * np.pi * np.outer(k256, k256) / 256
    c["c256"] = np.cos(a256).astype(np.float32)
    c["s256"] = np.sin(a256).astype(np.float32)
    c["tri"] = (np.arange(128)[:, None] < np.arange(128)[None, :]).astype(np.float32)
    sel = np.zeros((128, 64), np.float32)
    sel[64 + np.arange(64), np.arange(64)] = 1.0
    c["sel"] = sel
    pc = np.ones((2, 128, 16), np.float32)
    for ch in range(2):
        for g in range(2):
            w = (2, 4, 8, 16)[ch * 2 + g]
            for j in range(8):
                cnt = (j + w - w // 2) - max(j - w // 2, 0)
                pc[ch, g * 64:(g + 1) * 64, j] = w / cnt
                cnt2 = min(w - w // 2, 8 - j) + w // 2
                pc[ch, g * 64:(g + 1) * 64, 8 + j] = w / cnt2
    c["poolcorr"] = pc
    ms = np.zeros((5, 128, 5, 128), np.float32)
    dr_i = np.zeros((5, 128, 5, 128), np.int64)
    dc_i = np.zeros((5, 128, 5, 128), np.int64)
    kl = np.arange(128)
    for v, m in enumerate((0, 1, 2, 62, 63)):
        p0 = min(max(m - 2, 0), 59)
        for ch in range(5):
            krow = 2 * (p0 + ch) + kl // 64
            kcol = kl % 64
            qrow = 2 * m + kl // 64
            qcol = kl % 64
            rs_ = np.clip(qrow - 4, 0, 120)
            cs_ = np.clip(qcol - 8, 0, 48)
            okr = (krow[:, None] >= rs_[None, :]) & (krow[:, None] < rs_[None, :] + 8)
            okc = (kcol[:, None] >= cs_[None, :]) & (kcol[:, None] < cs_[None, :] + 16)
            ok = okr & okc
            dr = krow[:, None] - qrow[None, :] + 7
            dc = kcol[:, None] - qcol[None, :] + 15
            ms[v, :, ch, :] = np.where(ok, 0.0, -30000.0)
            dr_i[v, :, ch, :] = np.where(ok, dr, 0)
            dc_i[v, :, ch, :] = np.where(ok, dc, 0)
    c["namask"] = ms
    c["_dr"] = dr_i
    c["_dc"] = dc_i
    return c


_CONST = None


def _get_consts():
    global _CONST
    if _CONST is None:
        _CONST = _consts()
    return _CONST


CONST_NAMES = ("ident", "ropeC", "ropeS", "c128", "s128", "bdc64", "bds64", "c2", "s2", "c256", "s256",
               "tri", "sel", "poolcorr", "namask")


def _fm(v, nchunk):
    s = v.shape[:-1]
    return np.ascontiguousarray(np.swapaxes(v.reshape(*s, nchunk, 128), -1, -2))


def host_prep(inp, b, r=0):
    C = _get_consts()
    m = {}
    m["xin"] = np.ascontiguousarray(np.concatenate([inp["x"][b], inp["ctx"][b]], axis=0))
    cc = np.stack([inp["c"][b], inp["c_ctx"]], axis=-1)
    m["cc"] = np.ascontiguousarray(cc.reshape(8, 128, 2).transpose(1, 0, 2))
    m["ada_w"] = inp["ada_w"]
    m["ada_bT"] = _fm(inp["ada_b"], 48)
    m["n1g"] = _fm(inp["norm1_g"], 8)
    m["n2g"] = _fm(inp["norm2_g"], 8)
    m["fnorm"] = inp["final_norm"]
    w_in = inp["w_in"]
    if r == 1:
        perm = np.arange(w_in.shape[-1])
        for c0 in (0, 256, 512):
            perm[c0:c0 + 128], perm[c0 + 128:c0 + 256] = np.arange(c0 + 128, c0 + 256), np.arange(c0, c0 + 128)
        w_in = w_in[:, :, perm]
    m["w_in"] = w_in
    nb = inp["na_bias"][:, 2 * r:2 * r + 2]
    m["nab"] = np.ascontiguousarray(
        nb[:, :, C["_dr"], C["_dc"]].transpose(0, 2, 3, 1, 4, 5))
    pw = inp["pool_w"]
    bd = np.zeros((2, 2, 128, 128), np.float32)
    for l in range(2):
        for g in range(4):
            bd[l, g // 2, (g % 2) * 64:(g % 2 + 1) * 64, (g % 2) * 64:(g % 2 + 1) * 64] = pw[l, g]
    m["pool_wbd"] = bd
    m["pool_scT"] = _fm(inp["pool_scale"], 2)
    m["fno_w"] = inp["fno_w"]
    m["qng"] = _fm(inp["mla_q_norm"], 2)
    m["kvg"] = _fm(inp["mla_kv_norm"], 1)
    m["w_uq"] = inp["mla_w_uq"][:, :, 192 * r:192 * r + 192]
    m["w_uk"] = inp["mla_w_uk"][:, :, 128 * r:128 * r + 128]
    m["w_uv"] = inp["mla_w_uv"][:, :, 128 * r:128 * r + 128]
    m["gng"] = _fm(inp["grp_norm"].reshape(2, 1024), 8)
    m["w_out"] = inp["w_out"]
    eperm = list(range(8 * r, 8 * r + 8)) + list(range(8 * (1 - r), 8 * (1 - r) + 8))
    m["router_w"] = inp["router_w"][:, :, eperm]
    m["wg"] = inp["exp_w_gate"][:, 8 * r:8 * r + 8]
    m["wu"] = inp["exp_w_up"][:, 8 * r:8 * r + 8]
    m["wd"] = inp["exp_w_down"][:, 8 * r:8 * r + 8]
    m["rflag"] = np.full((128, 1), 1.0 if r == 0 else 0.0, np.float32)
    for k in CONST_NAMES:
        m[k] = C[k]
    return {k: np.ascontiguousarray(v, dtype=np.float32) for k, v in m.items()}


INPUT_SHAPES = {
    "xin": [TT, D], "cc": [128, 8, 2], "ada_w": [2, D, 6 * D], "ada_bT": [2, 128, 48], "n1g": [2, 128, 8],
    "n2g": [2, 128, 8], "fnorm": [D], "w_in": [2, D, 1696], "nab": [2, 5, 128, 2, 5, 128],
    "pool_wbd": [2, 2, 128, 128], "pool_scT": [2, 128, 2], "fno_w": [2, 256, 256], "qng": [2, 128, 2],
    "kvg": [2, 128, 1], "w_uq": [2, 256, 192], "w_uk": [2, 128, 128], "w_uv": [2, 128, 128], "gng": [2, 128, 8],
    "w_out": [2, D, D], "router_w": [2, D, 16], "wg": [2, 8, D, D], "wu": [2, 8, D, D], "wd": [2, 8, D, D], "rflag": [128, 1],
    "ident": [128, 128], "ropeC": [32, NL], "ropeS": [32, NL], "c128": [128, 128], "s128": [128, 128],
    "bdc64": [2, 128, 128], "bds64": [2, 128, 128], "c2": [64, 128, 64], "s2": [64, 128, 64],
    "c256": [256, 256], "s256": [256, 256], "tri": [128, 128], "sel": [128, 64], "poolcorr": [2, 128, 16],
    "namask": [5, 128, 5, 128],
}


class Ctx:
    pass


def build(stop=None, debug=(), layers=2):
    nc = bass.Bass("TRN2", target_bir_lowering=False)
    f = FW(nc)
    g = Ctx()
    g.nc, g.f = nc, f
    uid = [0]

    def dram(name, shape, dt=F32, loose=True):
        kind = "ExternalOutput" if name in debug else "Internal"
        return T(nc.dram_tensor(name, list(shape), dt, kind=kind), name, loose=loose)

    def sb(es, name, shape, dt=F32):
        uid[0] += 1
        return T(es.enter_context(nc.sbuf_tensor("%s_%d" % (name, uid[0]), list(shape), dt)), name)

    g.dram, g.sb = dram, sb
    I = {k: T(nc.dram_tensor(k, v, F32, kind="ExternalInput"), k) for k, v in INPUT_SHAPES.items()}
    g.I = I
    OUT = T(nc.dram_tensor("out", [NL, D], F32, kind="ExternalOutput"), "out", loose=True)
    g.OUT = OUT
    psb = [nc.alloc_psum_tensor("psb%d" % j, [128, 1024], F32) for j in range(4)]
    g.ps = [T(psb[i // 2][:, (i % 2) * 512:(i % 2 + 1) * 512], "ps%d" % i) for i in range(8)]
    g.ps2 = [T(psb[j][:, :], "ps2_%d" % j) for j in range(4)]

    S = {}
    S["QAT"] = dram("QAT", [128, TT], BF16)
    S["KAT"] = dram("KAT", [128, TT], BF16)
    S["VA"] = dram("VA", [TT, 2, 66], BF16)
    S["MAo"] = [dram("MAo%d" % k, [n * 128, 512], F32) for k, n in enumerate((8, 8, 1))]
    S["MAa"] = [dram("MAa%d" % k, [2 * n * 128, 512], F32) for k, n in enumerate((8, 8, 1))]
    S["PLT"] = dram("PLT", [256, TT], F32)
    S["FLT"] = dram("FLT", [256, TT], BF16)
    S["QT"] = dram("QT", [2, 96, TT], BF16)
    S["KT"] = dram("KT", [2, 96, TT], BF16)
    S["VD"] = dram("VD", [TT, 2, 128], BF16)
    S["MDo"] = [dram("MDo%d" % k, [n * 128, 512], F32) for k, n in enumerate((8, 8, 1))]
    S["MDa"] = [dram("MDa%d" % k, [2 * n * 128, 512], F32) for k, n in enumerate((8, 8, 1))]
    S["MIXT"] = dram("MIXT", [D, TT], F32)
    S["X1a"] = dram("X1a", [TT + 128, D], F32)
    S["X1b"] = dram("X1b", [TT + 128, D], F32)
    S["H2"] = dram("H2", [TT, RW], BF16)
    S["XS"] = dram("XS", [NEL * (CAP_L + CAP_C), RW], BF16)
    S["XG"] = [dram("XG%d" % k, [1024 if k < 16 else 512, D], F32) for k in range(NXCH)]
    S["X2"] = dram("X2", [TT, D], F32)
    S["AFFD"] = dram("AFFD", [TT + 128, NEXP], F32)
    S["BR"] = dram("BR", [128, 64, 256], BF16)
    S["BI"] = dram("BI", [128, 64, 256], BF16)
    S["DBG"] = dram("DBG", [128, 4096], F32)
    g.S = S

    with ExitStack() as ges:
        g.ident = sb(ges, "ident", [128, 128])
        g.identb = sb(ges, "identb", [128, 128], BF16)
        g.ones = sb(ges, "ones", [128, 128])
        g.onesb = sb(ges, "onesb", [128, 128], BF16)
        g.eps = sb(ges, "eps", [128, 1])
        g.mods = sb(ges, "mods", [128, 2, 6, 8, 2])
        g.A1 = sb(ges, "A1", [128, 2, 8, 2])
        g.A2 = sb(ges, "A2", [128, 2, 8, 2])
        g.AFF = sb(ges, "AFF", [128, NTILE, NEXP])
        g.IDX = sb(ges, "IDX", [128, NTILE, NEXP], I32)
        f.dma("sp", g.ident[:], I["ident"][:, :])
        f.copy("dve", g.identb[:], g.ident[:])
        f.memset("dve", g.ones[:], 1.0)
        f.memset("dve", g.onesb[:], 1.0)
        f.memset("dve", g.eps[:], EPS)

        g.breg = nc.gpsimd.to_reg(NEL * (CAP_L + CAP_C) - 1)
        g.breg2 = nc.gpsimd.to_reg(TT + 127)
        g.pidxi = sb(ges, "pidxi", [128, 1], I32)
        g.pidxb = sb(ges, "pidxb", [128, 1], BF16)
        g.dumi = sb(ges, "dumi", [128, 1], I32)
        g.rflag = sb(ges, "rflag", [128, 1])
        f.dma("sp", g.rflag[:], I["rflag"][:, :])
        f.op("pool", lambda E: E.iota(g.pidxi.h[:], pattern=[[0, 1]], base=0, channel_multiplier=1), [g.pidxi[:]], [])
        f.op("pool", lambda E: E.iota(g.dumi.h[:], pattern=[[0, 1]], base=TT, channel_multiplier=1), [g.dumi[:]], [])
        f.copy("dve", g.pidxb[:], g.pidxi[:])
        phase_adaln(g)
        f.barrier()
        if stop == "adaln":
            return finish(g)
        XCUR = I["xin"]
        for l in range(layers):
            last = (l == 1)
            phase_a(g, l, XCUR, from_xg=(l > 0))
            f.barrier()
            if stop == "a%d" % l:
                return finish(g)
            phase_na(g, l, not last)
            f.barrier()
            if stop == "na%d" % l:
                return finish(g)
            phase_pool(g, l, not last)
            f.barrier()
            if stop == "pool%d" % l:
                return finish(g)
            phase_fft(g, l, not last)
            f.barrier()
            if stop == "fft%d" % l:
                return finish(g)
            phase_mla(g, l, not last)
            f.barrier()
            if stop == "mla%d" % l:
                return finish(g)
            g.X1 = S["X1a"] if l == 0 else S["X1b"]
            phase_merge(g, l, XCUR, not last)
            f.barrier()
            if stop == "merge%d" % l:
                return finish(g)
            phase_route(g, l, not last)
            f.barrier()
            if stop == "route%d" % l:
                return finish(g)
            phase_dispatch(g, l, not last)
            f.barrier()
            if stop == "disp%d" % l:
                return finish(g)
            phase_experts(g, l, not last)
            f.barrier()
            if stop == "exp%d" % l:
                return finish(g)
            phase_exchange(g, l, not last)
            f.barrier()
            XCUR = S["X2"]
        phase_final(g)
        return finish(g)


def finish(g):
    g.f.barrier()
    return g.nc


def rstd_from_sum(g, out, in_, n, e_recip="dve"):
    f = g.f
    npart = _ap(out).shape[0]
    f.act(out, in_, AF.Sqrt, bias=g.eps[0:npart, :], scale=1.0 / n)
    f.recip(out, out)


def phase_adaln(g):
    f, I = g.f, g.I
    with ExitStack() as es:
        cc = g.sb(es, "cc", [128, 8, 2])
        sc = g.sb(es, "sc", [128, 8, 2])
        adab = g.sb(es, "adab", [128, 2, 48])
        n1g = g.sb(es, "n1g", [128, 2, 8])
        n2g = g.sb(es, "n2g", [128, 2, 8])
        aw = [g.sb(es, "aw%d" % i, [128, 8, 1024]) for i in range(2)]
        f.dma("sp", cc[:], I["cc"][:])
        for l in range(2):
            f.dma("sp", adab[:, l, :], I["ada_bT"][l])
            f.dma("sp", n1g[:, l, :], I["n1g"][l])
            f.dma("sp", n2g[:, l, :], I["n2g"][l])
        f.act(sc[:], cc[:], AF.Silu)
        k = 0
        for l in range(2):
            for grp in range(6):
                a = aw[k % 2]
                k += 1
                f.dma("sp", a[:], I["ada_w"][l, :, grp * 1024:(grp + 1) * 1024].rearrange("(j p) n -> p j n", p=128))
                ps = g.ps[k % 2]
                for j in range(8):
                    for kc in range(8):
                        f.matmul(ps[:, 2 * j:2 * j + 2], a[:, kc, j * 128:(j + 1) * 128], sc[:, kc, :],
                                 start=(kc == 0), stop=(kc == 7))
                for s in range(2):
                    f.tt("dve", g.mods[:, l, grp, :, s], ps[:, s:16:2], adab[:, l, grp * 8:(grp + 1) * 8], ALU.add)
            for s in range(2):
                f.stt("dve", g.A1[:, l, :, s], g.mods[:, l, 1, :, s], 1.0, n1g[:, l, :], ALU.add, ALU.mult)
                f.stt("dve", g.A2[:, l, :, s], g.mods[:, l, 4, :, s], 1.0, n2g[:, l, :], ALU.add, ALU.mult)


def bcast_vec(g, dst, vec8, tmp, psA, psB):
    f = g.f
    for j in range(8):
        f.ts("dve", tmp[:], g.ident[:], vec8[:, j:j + 1], None, ALU.mult)
        ps = psA if j < 4 else psB
        jj = j % 4
        f.matmul(ps[:, jj * 128:(jj + 1) * 128], g.ones[:], tmp[:], start=True, stop=True)
        if jj == 3:
            f.evac(dst[:, (j - 3) * 128:(j + 1) * 128], ps[:, :])


def tiles512():
    out = [(i * 512, 512, 0) for i in range(16)]
    out.append((NL, 256, 1))
    return out


def phase_a(g, l, X, from_xg=False):
    f, I, S = g.f, g.I, g.S
    with ExitStack() as es:
        sb = lambda n, s, d=F32: g.sb(es, n, s, d)
        win = sb("win", [128, 8, 1696], BF16)
        wkr = sb("wkr", [128, 8, 96], BF16)
        wkrs = sb("wkrs", [128, 8, 96], BF16)
        wuq = sb("wuq", [128, 2, 192], BF16)
        wuqs = sb("wuqs", [128, 2, 2, 96], BF16)
        wuk = sb("wuk", [128, 128], BF16)
        wuv = sb("wuv", [128, 128], BF16)
        qng = sb("qng", [128, 2])
        kvg = sb("kvg", [128, 1])
        for j in range(8):
            f.dma("pool", win[:, j, :], I["w_in"][l, j * 128:(j + 1) * 128, :])
        f.dma("pool", wuq[:], I["w_uq"][l].rearrange("(j p) n -> p j n", p=128))
        f.dma("pool", wuk[:], I["w_uk"][l])
        f.dma("pool", wuv[:], I["w_uv"][l])
        f.dma("sp", qng[:], I["qng"][l])
        f.dma("sp", kvg[:], I["kvg"][l])
        f.memset("dve", wkr[:], 0.0)
        f.memset("dve", wkrs[:], 0.0)
        f.memset("dve", wuqs[:], 0.0)
        f.copy("dve", wkr[:, :, 64:96], win[:, :, 1664:1696])
        for (d0, s0) in ((64, 1672), (72, 1664), (80, 1688), (88, 1680)):
            f.copy("dve", wkrs[:, :, d0:d0 + 8], win[:, :, s0:s0 + 8])
        for h in range(2):
            b0 = 96 * h + 64
            for (d0, s0) in ((64, b0 + 8), (72, b0), (80, b0 + 24), (88, b0 + 16)):
                f.copy("dve", wuqs[:, :, h, d0:d0 + 8], wuq[:, :, s0:s0 + 8])

        xt = [sb("xt%d" % i, [128, 4, 1024]) for i in range(2)]
        xu = [sb("xu%d" % i, [128, 4, 1024]) for i in range(2)] if from_xg else None
        xn = sb("xn", [128, 4, 1024])
        junk = sb("junk", [128, 1024], BF16)
        ss = sb("ss", [128, 4])
        rstd = sb("rstd", [128, 4])
        hT = [sb("hT%d" % i, [128, 8, 512], BF16) for i in range(2)]
        stg = [sb("stg%d" % i, [128, 4, 512], BF16) for i in range(2)]
        plst = [sb("plst%d" % i, [128, 2, 512]) for i in range(2)]
        qc = sb("qc", [128, 2, 512])
        kvc = sb("kvc", [128, 512])
        sq = sb("sq", [128, 3, 512])
        rq = sb("rq", [128, 512])
        rk = sb("rk", [128, 512])
        qn = sb("qn", [128, 2, 512], BF16)
        ckv = sb("ckv", [128, 512], BF16)
        rc = [sb("rc%d" % i, [96, 512]) for i in range(2)]
        rs = [sb("rs%d" % i, [96, 512]) for i in range(2)]
        t1 = sb("t1", [96, 512])
        t2 = sb("t2", [96, 512])
        qst = [sb("qst%d" % i, [96, 2, 512], BF16) for i in range(2)]
        kst = [sb("kst%d" % i, [96, 2, 512], BF16) for i in range(2)]
        vast = [sb("vast%d" % i, [128, 4, 2, 66], BF16) for i in range(2)]
        vdst = [sb("vdst%d" % i, [128, 4, 2, 128], BF16) for i in range(2)]
        for i in range(2):
            f.memset("pool", vast[i][:], 1.0)
            f.memset("pool", vdst[i][:], 1.0)
        rot = [2]

        def nxt():
            p = g.ps[rot[0]]
            rot[0] = 2 + (rot[0] - 2 + 1) % 6
            return p

        tl = tiles512()

        def load(i):
            t0, tw, c = tl[i]
            ns = tw // 128
            if from_xg:
                f.dma("sp", xt[i % 2][:, 0:ns, :], S["XG"][i][0:tw, :].rearrange("(s p) d -> p s d", p=128))
                f.dma("sp", xu[i % 2][:, 0:ns, :], S["XG"][i][tw:2 * tw, :].rearrange("(s p) d -> p s d", p=128))
            else:
                f.dma("sp", xt[i % 2][:, 0:ns, :], X[t0:t0 + tw, :].rearrange("(s p) d -> p s d", p=128))
            if c == 0:
                f.dma("sp", rc[i % 2][64:96, :], I["ropeC"][:, t0:t0 + 512])
                f.dma("sp", rs[i % 2][64:96, :], I["ropeS"][:, t0:t0 + 512])

        load(0)
        for i, (t0, tw, c) in enumerate(tl):
            ns = tw // 128
            if i + 1 < len(tl):
                load(i + 1)
            x_ = xt[i % 2]
            h_ = hT[i % 2]
            st_ = stg[i % 2]
            pl_ = plst[i % 2]
            if from_xg:
                f.tt("dve", x_[:, 0:ns, :], x_[:, 0:ns, :], xu[i % 2][:, 0:ns, :], ALU.add)
                f.dma("pool", S["X2"][t0:t0 + tw, :].rearrange("(s p) d -> p s d", p=128), x_[:, 0:ns, :])
            for s in range(ns):
                f.act(junk[:], x_[:, s, :], AF.Square, accum_out=ss[:, s:s + 1])
            rstd_from_sum(g, rstd[:, 0:ns], ss[:, 0:ns], 1024)
            for s in range(ns):
                if s % 2:
                    f.act(xn[:, s, :], x_[:, s, :], AF.Identity, scale=rstd[:, s:s + 1])
                else:
                    f.ts("dve", xn[:, s, :], x_[:, s, :], rstd[:, s:s + 1], None, ALU.mult)
            for j in range(8):
                ps = g.ps[j % 2]
                for s in range(ns):
                    f.transpose(ps[:, s * 128:(s + 1) * 128], xn[:, s, j * 128:(j + 1) * 128], g.ident[:])
                a_ = g.A1[:, l, j, c:c + 1]
                b_ = g.mods[:, l, 0, j, c:c + 1]
                if j % 2:
                    f.act(h_[:, j, 0:tw], ps[:, 0:tw], AF.Identity, bias=b_, scale=a_)
                else:
                    f.ts("dve", h_[:, j, 0:tw], ps[:, 0:tw], a_, b_, ALU.mult, ALU.add)

            def proj(ps, lhs_fn, m=128):
                for kc in range(8):
                    f.matmul(ps[0:m, 0:tw], lhs_fn(kc), h_[:, kc, 0:tw], start=(kc == 0), stop=(kc == 7))

            for ci, c0 in enumerate((0, 256, 1024, 1152)):
                ps = nxt()
                proj(ps, lambda kc: win[:, kc, c0:c0 + 128])
                if ci < 1:
                    f.act(st_[:, ci, 0:tw], ps[:, 0:tw], AF.Copy, scale=0.125)
                else:
                    f.evac(st_[:, ci, 0:tw], ps[:, 0:tw])
            f.dma("pool", S["QAT"][:, t0:t0 + tw], st_[:, 0, 0:tw])
            f.dma("pool", S["KAT"][:, t0:t0 + tw], st_[:, 1, 0:tw])
            f.dma("pool", S["FLT"][:, t0:t0 + tw].rearrange("(c p) t -> p c t", p=128), st_[:, 2:4, 0:tw])
            for ci, c0 in enumerate((768, 896)):
                ps = nxt()
                proj(ps, lambda kc: win[:, kc, c0:c0 + 128])
                f.evac(pl_[:, ci, 0:tw], ps[:, 0:tw])
            f.dma("pool", S["PLT"][:, t0:t0 + tw].rearrange("(c p) t -> p c t", p=128), pl_[:, :, 0:tw])
            for ci, c0 in enumerate((1280, 1408)):
                ps = nxt()
                proj(ps, lambda kc: win[:, kc, c0:c0 + 128])
                f.evac(qc[:, ci, 0:tw], ps[:, 0:tw])
            ps = nxt()
            proj(ps, lambda kc: win[:, kc, 1536:1664])
            f.evac(kvc[:, 0:tw], ps[:, 0:tw])
            va_ = vast[i % 2]
            for s in range(ns):
                ps = nxt()
                for kc in range(8):
                    f.matmul(ps[:, 0:128], h_[:, kc, s * 128:(s + 1) * 128], win[:, kc, 512:640],
                             start=(kc == 0), stop=(kc == 7))
                f.evac(va_[:, s, :, 0:64], ps[:, 0:128].rearrange("p (h d) -> p h d", h=2))
            f.dma("pool", S["VA"][t0:t0 + tw].rearrange("(s p) h c -> p s h c", p=128), va_[:, 0:ns])
            k_ = kst[i % 2]
            pkr = nxt()
            proj(pkr, lambda kc: wkr[:, kc, :], m=96)
            if c == 0:
                pkrs = nxt()
                proj(pkrs, lambda kc: wkrs[:, kc, :], m=96)
                f.tt("dve", t1[64:96, 0:tw], pkr[64:96, 0:tw], rc[i % 2][64:96, 0:tw], ALU.mult)
                f.tt("dve", t2[64:96, 0:tw], pkrs[64:96, 0:tw], rs[i % 2][64:96, 0:tw], ALU.mult)
                f.tt("pool", k_[64:96, 0, 0:tw], t1[64:96, 0:tw], t2[64:96, 0:tw], ALU.add)
            else:
                f.evac(k_[64:96, 0, 0:tw], pkr[64:96, 0:tw])
            for h in range(1, 2):
                f.copy("pool", k_[64:96, h, 0:tw], k_[64:96, 0, 0:tw])
            for ci in range(2):
                f.act(sq[:, ci, 0:tw], qc[:, ci, 0:tw], AF.Square)
            f.act(sq[:, 2, 0:tw], kvc[:, 0:tw], AF.Square)
            psum_q = nxt()
            for ci in range(2):
                f.matmul(psum_q[:, 0:tw], g.ones[:], sq[:, ci, 0:tw], start=(ci == 0), stop=(ci == 1))
            rstd_from_sum(g, rq[:, 0:tw], psum_q[:, 0:tw], 256)
            psum_k = nxt()
            f.matmul(psum_k[:, 0:tw], g.ones[:], sq[:, 2, 0:tw], start=True, stop=True)
            rstd_from_sum(g, rk[:, 0:tw], psum_k[:, 0:tw], 128)
            for ci in range(2):
                f.stt("dve", qn[:, ci, 0:tw], qc[:, ci, 0:tw], qng[:, ci:ci + 1], rq[:, 0:tw], ALU.mult, ALU.mult)
            f.stt("dve", ckv[:, 0:tw], kvc[:, 0:tw], kvg[:, 0:1], rk[:, 0:tw], ALU.mult, ALU.mult)
            for h in range(2):
                ps = nxt()
                f.matmul(ps[0:64, 0:tw], wuk[:, 64 * h:64 * h + 64], ckv[:, 0:tw], start=True, stop=True)
                f.evac(k_[0:64, h, 0:tw], ps[0:64, 0:tw])
            f.dma("pool", S["KT"][:, :, t0:t0 + tw].rearrange("h r t -> r h t"), k_[:, :, 0:tw])
            vd_ = vdst[i % 2]
            for s in range(ns):
                ps = nxt()
                f.matmul(ps[:, 0:128], ckv[:, s * 128:(s + 1) * 128], wuv[:], start=True, stop=True)
                f.evac(vd_[:, s, :, 0:64], ps[:, 0:128].rearrange("p (h d) -> p h d", h=2))
            f.dma("pool", S["VD"][t0:t0 + tw].rearrange("(s p) h c -> p s h c", p=128), vd_[:, 0:ns])
            q_ = qst[i % 2]
            for h in range(2):
                ps = nxt()
                for ci in range(2):
                    f.matmul(ps[0:96, 0:tw], wuq[:, ci, 96 * h:96 * h + 96], qn[:, ci, 0:tw],
                             start=(ci == 0), stop=(ci == 1))
                f.copy("act", q_[0:64, h, 0:tw], ps[0:64, 0:tw])
                if c == 0:
                    ps2 = nxt()
                    for ci in range(2):
                        f.matmul(ps2[0:96, 0:tw], wuqs[:, ci, h, :], qn[:, ci, 0:tw], start=(ci == 0), stop=(ci == 1))
                    f.tt("dve", t1[64:96, 0:tw], ps[64:96, 0:tw], rc[i % 2][64:96, 0:tw], ALU.mult)
                    f.tt("dve", t2[64:96, 0:tw], ps2[64:96, 0:tw], rs[i % 2][64:96, 0:tw], ALU.mult)
                    f.tt("pool", q_[64:96, h, 0:tw], t1[64:96, 0:tw], t2[64:96, 0:tw], ALU.add)
                else:
                    f.copy("act", q_[64:96, h, 0:tw], ps[64:96, 0:tw])
            f.dma("pool", S["QT"][:, :, t0:t0 + tw].rearrange("h r t -> r h t"), q_[:, :, 0:tw])


def phase_na(g, l, with_ctx):
    f, I, S = g.f, g.I, g.S
    with ExitStack() as es:
        sb = lambda n, s, d=F32: g.sb(es, n, s, d)
        KA = sb("KA", [128, 1, TT], BF16)
        QA = sb("QA", [128, 1, TT], BF16)
        VA = sb("VAs", [128, NTILE, 2, 66], BF16)
        BT = sb("BT", [128, 5, 2, 5, 128], BF16)
        f.dma("sp", KA[:, 0, :], S["KAT"][:, :])
        f.dma("sp", QA[:, 0, :], S["QAT"][:, :])
        for q4 in range(0, NTILE, 11):
            f.dma("sp", VA[:, q4:q4 + 11], S["VA"][q4 * 128:(q4 + 11) * 128].rearrange("(t p) h c -> p t h c", p=128))
        btmp = sb("btmp", [128, 2, 5, 128])
        mtmp = sb("mtmp", [128, 5, 128])
        for v in range(5):
            f.dma("sp", btmp[:], I["nab"][l, v])
            f.dma("sp", mtmp[:], I["namask"][v])
            for h in range(2):
                f.tt("dve", BT[:, v, h], btmp[:, h], mtmp[:], ALU.add)
        PT = [sb("PT%d" % i, [128, 896], BF16) for i in range(3)]
        atok = [sb("atok%d" % i, [128, 128]) for i in range(2)]
        rden = sb("rden", [128, 2])
        aT = [sb("aT%d" % i, [128, 512]) for i in range(2)]
        nblk = 64 + (2 if with_ctx else 0)
        steps = []
        for m in range(nblk):
            isctx = m >= 64
            if not isctx:
                v = {0: 0, 1: 1, 62: 3, 63: 4}.get(m, 2)
                p0 = min(max(m - 2, 0), 59)
                chunks = [(p0 + c) for c in range(5)] + [64, 65]
            else:
                v = 0
                chunks = [64, 65]
            for h in range(2):
                steps.append((m, h, isctx, v, chunks))

        def emit_s(it):
            m, h, isctx, v, chunks = steps[it]
            nch = len(chunks)
            ch, pb = 0, 64 * h
            sA = g.ps[(it * 3) % 6]
            sB = g.ps[(it * 3 + 1) % 6]
            pt_ = PT[it % 3]
            q_ = QA[pb:pb + 64, ch, m * 128:(m + 1) * 128]
            for ci, kt in enumerate(chunks):
                dst = sA[:, ci * 128:(ci + 1) * 128] if ci < 4 else sB[:, (ci - 4) * 128:(ci - 3) * 128]
                bias = (not isctx) and ci < 5
                f.matmul(dst, KA[pb:pb + 64, ch, kt * 128:(kt + 1) * 128], q_, start=True, stop=not bias)
                if bias:
                    f.matmul(dst, g.identb[:], BT[:, v, h, ci, :], start=False, stop=True)
            na = min(nch, 4) * 128
            f.act(pt_[:, 0:na], sA[:, 0:na], AF.Exp)
            if nch > 4:
                nb_ = (nch - 4) * 128
                f.act(pt_[:, 512:512 + nb_], sB[:, 0:nb_], AF.Exp)

        def emit_pv(it):
            m, h, isctx, v, chunks = steps[it]
            nch = len(chunks)
            oP = g.ps[(it * 3 + 2) % 6]
            pt_ = PT[it % 3]
            at_ = atok[m % 2]
            for ci, kt in enumerate(chunks):
                src = pt_[:, ci * 128:(ci + 1) * 128] if ci < 4 else pt_[:, 512 + (ci - 4) * 128:512 + (ci - 3) * 128]
                f.matmul(oP[:, 0:65], src, VA[:, kt, h, 0:65], start=(ci == 0), stop=(ci == nch - 1))
            f.recip(rden[:, h:h + 1], oP[:, 64:65])
            f.ts("dve", at_[:, h * 64:(h + 1) * 64], oP[:, 0:64], rden[:, h:h + 1], None, ALU.mult)
            if h == 1:
                o_ = aT[(m // 4) % 2]
                tp = g.ps[6 + (m % 2)]
                f.transpose(tp[:, 0:128], at_[:, 0:128], g.ident[:])
                f.evac(o_[:, (m % 4) * 128:(m % 4 + 1) * 128], tp[:, 0:128])
                if m % 4 == 3 or m == nblk - 1:
                    tile_i = m // 4
                    w = (m - tile_i * 4 + 1) * 128
                    f.dma("pool", S["MAo"][tile_i // 8][(tile_i % 8) * 128:(tile_i % 8 + 1) * 128, 0:w], o_[:, 0:w])

        n = len(steps)
        for it in range(n + 1):
            if it < n:
                emit_s(it)
            if it >= 1:
                emit_pv(it - 1)
        for k_ in range(3 if with_ctx else 2):
            f.collective(S["MAa"][k_][:, :], S["MAo"][k_][:, :], lambda E, k_=k_: E.collective_compute(
                "AllGather", ALU.bypass, replica_groups=[[0, 1], [2, 3], [4, 5], [6, 7]],
                ins=[S["MAo"][k_].h.ap().opt()], outs=[S["MAa"][k_].h.ap().opt()]))


def phase_pool(g, l, with_ctx):
    f, I, S = g.f, g.I, g.S
    with ExitStack() as es:
        sb = lambda n, s, d=F32: g.sb(es, n, s, d)
        wbd = sb("wbd", [128, 2, 128], BF16)
        psc = sb("psc", [128, 2])
        corr = sb("corr", [128, 2, 16])
        for c in range(2):
            f.dma("pool", wbd[:, c, :], I["pool_wbd"][l, c])
            f.dma("sp", corr[:, c, :], I["poolcorr"][c])
        f.dma("sp", psc[:], I["pool_scT"][l])
        L = NL + 16
        u = sb("u", [128, L])
        a = sb("sa", [128, L])
        b = sb("sbb", [128, L])
        pooled = sb("pooled", [128, NL], BF16)
        yst = [sb("yst%d" % i, [128, 512]) for i in range(2)]
        seqs = [(0, NL)] + ([(NL, NC_)] if with_ctx else [])
        k = 0
        for (t0, n) in seqs:
            for c in range(2):
                Ln = n + 16
                f.memset("pool", u[:, 0:8], 0.0)
                f.memset("pool", u[:, 8 + n:16 + n], 0.0)
                f.dma("sp", u[:, 8:8 + n], S["PLT"][c * 128:(c + 1) * 128, t0:t0 + n])
                f.tt("dve", a[:, 0:Ln - 1], u[:, 0:Ln - 1], u[:, 1:Ln], ALU.add)
                if c == 0:
                    f.ts("dve", b[0:64, 8:8 + n], a[0:64, 7:7 + n], 0.5, None, ALU.mult)
                    f.stt("dve", b[64:128, 8:8 + n], a[64:128, 6:6 + n], 1.0, a[64:128, 8:8 + n], ALU.mult, ALU.add)
                    f.ts("dve", b[64:128, 8:8 + n], b[64:128, 8:8 + n], 0.25, None, ALU.mult)
                else:
                    f.tt("dve", b[:, 0:Ln - 3], a[:, 0:Ln - 3], a[:, 2:Ln - 1], ALU.add)
                    f.tt("dve", a[:, 0:Ln - 7], b[:, 0:Ln - 7], b[:, 4:Ln - 3], ALU.add)
                    f.stt("dve", b[64:128, 8:8 + n], a[64:128, 0:n], 1.0, a[64:128, 8:8 + n], ALU.mult, ALU.add)
                    f.ts("dve", b[64:128, 8:8 + n], b[64:128, 8:8 + n], 1.0 / 16, None, ALU.mult)
                    f.ts("dve", b[0:64, 8:8 + n], a[0:64, 4:4 + n], 0.125, None, ALU.mult)
                f.tt("dve", b[:, 8:16], b[:, 8:16], corr[:, c, 0:8], ALU.mult)
                f.tt("dve", b[:, n:8 + n], b[:, n:8 + n], corr[:, c, 8:16], ALU.mult)
                f.tt("dve", pooled[:, 0:n], b[:, 8:8 + n], u[:, 8:8 + n], ALU.subtract)
                for q0 in range(0, n, 512):
                    w = min(512, n - q0)
                    ps = g.ps[k % 4]
                    y_ = yst[k % 2]
                    k += 1
                    f.matmul(ps[:, 0:w], wbd[:, c, :], pooled[:, q0:q0 + w], start=True, stop=True)
                    f.ts("dve", y_[:, 0:w], ps[:, 0:w], psc[:, c:c + 1], None, ALU.mult)
                    f.dma("pool", S["MIXT"][256 + c * 128:256 + (c + 1) * 128, t0 + q0:t0 + q0 + w], y_[:, 0:w])


def phase_fft(g, l, with_ctx):
    f, I, S = g.f, g.I, g.S
    sc_l = 1.0 / math.sqrt(NL * 64.0)
    sc_c = 1.0 / math.sqrt(NC_ * 64.0)
    with ExitStack() as es:
        sb = lambda n, s, d=F32: g.sb(es, n, s, d)
        FL = sb("FL", [128, 2, TT], BF16)
        for c in range(2):
            f.dma("sp", FL[:, c, :], S["FLT"][c * 128:(c + 1) * 128, :])
        fno = sb("fno", [128, 2, 256])
        bdc = sb("bdc", [128, 2, 128])
        bds = sb("bds", [128, 2, 128])
        f.dma("sp", fno[:], I["fno_w"][l].rearrange("(c p) n -> p c n", p=128))
        f.dma("sp", bdc[:], I["bdc64"][:].rearrange("c p n -> p c n"))
        f.dma("sp", bds[:], I["bds64"][:].rearrange("c p n -> p c n"))
        Wc = sb("Wc", [128, 2, 256], BF16)
        Wsn = sb("Wsn", [128, 2, 256], BF16)
        Wcc = sb("Wcc", [128, 2, 256], BF16)
        Wsnc = sb("Wsnc", [128, 2, 256], BF16)
        for c in range(2):
            ps = g.ps[c]
            f.matmul(ps[:, 0:256], bdc[:, c, :], fno[:, c, :], start=True, stop=True)
            f.act(Wc[:, c, :], ps[:, 0:256], AF.Copy, scale=sc_l)
            f.act(Wcc[:, c, :], ps[:, 0:256], AF.Copy, scale=sc_c)
            ps = g.ps[2 + c]
            f.matmul(ps[:, 0:256], bds[:, c, :], fno[:, c, :], start=True, stop=True)
            f.act(Wsn[:, c, :], ps[:, 0:256], AF.Copy, scale=-sc_l)
            f.act(Wsnc[:, c, :], ps[:, 0:256], AF.Copy, scale=-sc_c)
        c128 = sb("c128", [128, 128], BF16)
        s128 = sb("s128", [128, 128], BF16)
        s128n = sb("s128n", [128, 128], BF16)
        f.dma("pool", c128[:], I["c128"][:, :])
        f.dma("pool", s128[:], I["s128"][:, :])
        f.ts("dve", s128n[:], s128[:], -1.0, None, ALU.mult)
        with ExitStack() as es1:
            Ar = g.sb(es1, "Ar", [128, 64, 256], BF16)
            Ai = g.sb(es1, "Ai", [128, 64, 256], BF16)
            bst = [g.sb(es1, "bst%d" % i, [128, 2, 2, 256], BF16) for i in range(4)]
            k = 0
            for n2 in range(0, 64, 2):
                for (W_, A_) in ((Wc, Ar), (Wsn, Ai)):
                    ps = g.ps[k % 4]
                    k += 1
                    for j in range(2):
                        for c in range(2):
                            f.matmul(ps[:, j * 256:(j + 1) * 256], FL[:, c, n2 + j:NL:64], W_[:, c, :],
                                     start=(c == 0), stop=(c == 1))
                    f.evac(A_[:, n2:n2 + 2, :], ps[:, :].rearrange("p (j n) -> p j n", j=2))
            for blk in range(32):
                n2 = 2 * blk
                pr = g.ps[4 + (blk % 2) * 2]
                pi = g.ps[5 + (blk % 2) * 2]
                rr = Ar[:, n2:n2 + 2, :]
                ri = Ai[:, n2:n2 + 2, :]
                f.matmul(pr[:, :], c128[:], rr, start=True, stop=False)
                f.matmul(pr[:, :], s128[:], ri, start=False, stop=True)
                f.matmul(pi[:, :], c128[:], ri, start=True, stop=False)
                f.matmul(pi[:, :], s128n[:], rr, start=False, stop=True)
                b_ = bst[blk % 4]
                f.copy("act", b_[:, 0], pr[:, :].rearrange("p (j n) -> p j n", j=2))
                f.copy("dve", b_[:, 1], pi[:, :].rearrange("p (j n) -> p j n", j=2))
                f.dma("pool", S["BR"][:, n2:n2 + 2, :], b_[:, 0])
                f.dma("pool", S["BI"][:, n2:n2 + 2, :], b_[:, 1])
        f.barrier()
        with ExitStack() as es2:
            BrT = g.sb(es2, "BrT", [64, 128, 128], BF16)
            BiT = g.sb(es2, "BiT", [64, 128, 128], BF16)
            c2 = g.sb(es2, "c2", [64, 128, 64], BF16)
            s2 = g.sb(es2, "s2", [64, 128, 64], BF16)
            cT = g.sb(es2, "cT", [128, NL])
            f.dma("pool", c2[:], I["c2"][:])
            f.dma("pool", s2[:], I["s2"][:])
            k = 0
            for half in range(2):
                hs = slice(half * 128, (half + 1) * 128)
                for q in range(4):
                    f.dma("sp", BrT[:, q * 32:(q + 1) * 32, :], S["BR"][q * 32:(q + 1) * 32, :, hs].rearrange("k n c -> n k c"))
                    f.dma("sp", BiT[:, q * 32:(q + 1) * 32, :], S["BI"][q * 32:(q + 1) * 32, :, hs].rearrange("k n c -> n k c"))
                for k1_0 in range(0, 128, 8):
                    ps = g.ps[k % 4]
                    k += 1
                    for j in range(8):
                        k1 = k1_0 + j
                        f.matmul(ps[:, j * 64:(j + 1) * 64], BrT[:, k1, :], c2[:, k1, :], start=True, stop=False)
                        f.matmul(ps[:, j * 64:(j + 1) * 64], BiT[:, k1, :], s2[:, k1, :], start=False, stop=True)
                    dst = cT[:, :].rearrange("p (k2 k1) -> p k2 k1", k1=128)[:, :, k1_0:k1_0 + 8]
                    f.evac(dst, ps[:, :].rearrange("p (j k2) -> p k2 j", j=8))
                f.dma("pool", S["MIXT"][512 + half * 128:512 + (half + 1) * 128, 0:NL], cT[:, :])
        if with_ctx:
            f.barrier()
            with ExitStack() as es3:
                c256 = g.sb(es3, "c256", [128, 2, 256], BF16)
                s256 = g.sb(es3, "s256", [128, 2, 256], BF16)
                f.dma("pool", c256[:], I["c256"][:].rearrange("(c p) n -> p c n", p=128))
                f.dma("pool", s256[:], I["s256"][:].rearrange("(c p) n -> p c n", p=128))
                Acr = g.sb(es3, "Acr", [128, 2, 256], BF16)
                Aci = g.sb(es3, "Aci", [128, 2, 256], BF16)
                ccT = g.sb(es3, "ccT", [128, 2, 256])
                for nt in range(2):
                    for wi, (W_, A_) in enumerate(((Wcc, Acr), (Wsnc, Aci))):
                        ps = g.ps[nt * 2 + wi]
                        for c in range(2):
                            f.matmul(ps[:, 0:256], FL[:, c, NL + nt * 128:NL + (nt + 1) * 128], W_[:, c, :],
                                     start=(c == 0), stop=(c == 1))
                        f.evac(A_[:, nt, :], ps[:, 0:256])
                for half in range(2):
                    ps = g.ps[4 + half]
                    i = 0
                    for nt in range(2):
                        for (A_, M_) in ((Acr, c256), (Aci, s256)):
                            f.matmul(ps[:, 0:256], A_[:, nt, half * 128:(half + 1) * 128], M_[:, nt, :],
                                     start=(i == 0), stop=(i == 3))
                            i += 1
                    f.evac(ccT[:, half, :], ps[:, 0:256])
                    f.dma("pool", S["MIXT"][512 + half * 128:512 + (half + 1) * 128, NL:TT], ccT[:, half, :])


def phase_mla(g, l, with_ctx):
    f, I, S = g.f, g.I, g.S
    scale = 96.0 ** -0.5
    with ExitStack() as es:
        sb = lambda n, s, d=F32: g.sb(es, n, s, d)
        KT = [sb("KTs%d" % i, [96, TT], BF16) for i in range(2)]
        VD = [sb("VDs%d" % i, [128, NTILE, 128], BF16) for i in range(2)]
        qs = [sb("qs%d" % i, [96, 512], BF16) for i in range(2)]
        P = [sb("P%d" % i, [128, 1024], BF16) for i in range(3)]
        osb = sb("osb", [128, 512])
        rdn = sb("rdn", [64, 512])
        dT = [sb("dT%d" % i, [64, 512]) for i in range(2)]
        sel = sb("sel", [128, 64])
        f.dma("sp", sel[:], I["sel"][:, :])
        qtiles = [(i * 512, 512, False) for i in range(16)] + ([(NL, 256, True)] if with_ctx else [])

        def loadkv(h):
            f.dma("sp", KT[h % 2][:, :], S["KT"][h])
            for q in range(0, NTILE, 22):
                f.dma("sp", VD[h % 2][:, q:q + 22, :],
                      S["VD"][q * 128:(q + 22) * 128, h, :].rearrange("(t p) c -> p t c", p=128))

        loadkv(0)
        it = 0
        for h in range(2):
            if h + 1 < 2:
                loadkv(h + 1)
            K_ = KT[h % 2]
            V_ = VD[h % 2]
            f.dma("sp", qs[0][:, 0:512], S["QT"][h, :, 0:512])
            for qi, (t0, tw, isctx) in enumerate(qtiles):
                if qi + 1 < len(qtiles):
                    n0, nw, _ = qtiles[qi + 1]
                    f.dma("sp", qs[(qi + 1) % 2][:, 0:nw], S["QT"][h, :, n0:n0 + nw])
                q_ = qs[qi % 2]
                kts = [64, 65] if isctx else list(range(NTILE))
                oP = g.ps[6 + (qi % 2)]
                npair = len(kts) // 2
                base = it
                for i in range(npair + 1):
                    if i < npair:
                        sP = g.ps2[(base + i) % 2]
                        p_ = P[(base + i) % 3]
                        for hh in range(2):
                            kt = kts[2 * i + hh]
                            f.matmul(sP[:, hh * 512:hh * 512 + tw], K_[:, kt * 128:(kt + 1) * 128], q_[:, 0:tw],
                                     start=True, stop=True)
                        if tw == 512:
                            f.act(p_[:, :], sP[:, :], AF.Exp, scale=scale)
                        else:
                            for hh in range(2):
                                f.act(p_[:, hh * 512:hh * 512 + tw], sP[:, hh * 512:hh * 512 + tw], AF.Exp, scale=scale)
                    j = i - 1
                    if j >= 0:
                        p_ = P[(base + j) % 3]
                        for hh in range(2):
                            kk = 2 * j + hh
                            f.matmul(oP[:, 0:tw], V_[:, kts[kk], :], p_[:, hh * 512:hh * 512 + tw],
                                     start=(kk == 0), stop=(kk == len(kts) - 1))
                it += npair
                f.copy("dve", osb[:, 0:tw], oP[:, 0:tw])
                dP = g.ps[4 + (qi % 2)]
                f.matmul(dP[0:64, 0:tw], sel[:], osb[:, 0:tw], start=True, stop=True)
                f.recip(rdn[:, 0:tw], dP[0:64, 0:tw])
                d_ = dT[qi % 2]
                f.tt("dve", d_[:, 0:tw], osb[0:64, 0:tw], rdn[:, 0:tw], ALU.mult)
                f.dma("pool", S["MDo"][qi // 8][(qi % 8) * 128 + h * 64:(qi % 8) * 128 + (h + 1) * 64, 0:tw], d_[:, 0:tw])
        nck = 3 if with_ctx else 2
        for k_ in range(nck):
            f.collective(S["MDa"][k_][:, :], S["MDo"][k_][:, :], lambda E, k_=k_: E.collective_compute(
                "AllGather", ALU.bypass, replica_groups=[[0, 1], [2, 3], [4, 5], [6, 7]],
                ins=[S["MDo"][k_].h.ap().opt()], outs=[S["MDa"][k_].h.ap().opt()]))


def phase_merge(g, l, X, with_ctx):
    f, I, S = g.f, g.I, g.S
    with ExitStack() as es:
        sb = lambda n, s, d=F32: g.sb(es, n, s, d)
        ncv = 2 if with_ctx else 1
        Wo = [sb("Wo%d" % c, [128, 8, 1024], BF16) for c in range(ncv)]
        A2b = [sb("A2b%d" % c, [128, 1024]) for c in range(ncv)]
        B2b = [sb("B2b%d" % c, [128, 1024]) for c in range(ncv)]
        rw = sb("rw", [128, 8, 16])
        gng = sb("gng", [128, 8])
        f.dma("sp", rw[:], I["router_w"][l].rearrange("(j p) n -> p j n", p=128))
        f.dma("sp", gng[:], I["gng"][l])
        with ExitStack() as es0:
            gtb = [g.sb(es0, "gtb%d" % c, [128, 1024]) for c in range(ncv)]
            tmpd = g.sb(es0, "tmpd", [128, 128])
            for c in range(ncv):
                bcast_vec(g, gtb[c], g.mods[:, l, 2, :, c], tmpd, g.ps[6], g.ps[7])
                bcast_vec(g, A2b[c], g.A2[:, l, :, c], tmpd, g.ps[6], g.ps[7])
                bcast_vec(g, B2b[c], g.mods[:, l, 3, :, c], tmpd, g.ps[6], g.ps[7])
            wst = [g.sb(es0, "wst%d" % i, [128, 1024]) for i in range(2)]
            for kc in range(8):
                w_ = wst[kc % 2]
                f.dma("sp", w_[:], I["w_out"][l, kc * 128:(kc + 1) * 128, :])
                for c in range(ncv):
                    f.tt("dve" if c == 0 else "pool", Wo[c][:, kc, :], w_[:], gtb[c][:], ALU.mult)
            f.barrier()
        mx = [sb("mx%d" % i, [128, 8, 512]) for i in range(2)]
        x1 = [sb("x1%d" % i, [128, 4, 1024]) for i in range(3)]
        sqt = sb("sqt", [128, 2, 512])
        rg = sb("rg", [128, 512])
        catT = sb("catT", [128, 8, 512], BF16)
        xn2 = sb("xn2", [128, 4, 1024])
        h2row = sb("h2row", [128, 4, RW], BF16)
        f.memset("pool", h2row[:], 0.0)
        for s_ in range(4):
            f.copy("pool", h2row[:, s_, 1025:1026], g.pidxb[:])
        h2T = sb("h2T", [128, 8, 512])
        tmp = sb("tmp", [128, 1024])
        junk = sb("junk", [128, 1024], BF16)
        ss = sb("ss2", [128, 4])
        rstd = sb("rstd2", [128, 4])
        ex = sb("ex", [128, 4, 16])
        se = sb("se", [128, 4])
        tl = [t for t in tiles512() if (with_ctx or t[2] == 0)]

        def load(i):
            t0, tw, c = tl[i]
            ns = tw // 128
            f.dma("sp", mx[i % 2][:, 2:6, 0:tw], S["MIXT"][256:768, t0:t0 + tw].rearrange("(j p) t -> p j t", p=128))
            ck, n_ = i // 8, (8, 8, 1)[i // 8]
            for rr in range(2):
                r0 = (rr * n_ + (i % 8)) * 128
                f.dma("sp", mx[i % 2][:, rr, 0:tw], S["MAa"][ck][r0:r0 + 128, 0:tw])
                f.dma("sp", mx[i % 2][:, 6 + rr, 0:tw], S["MDa"][ck][r0:r0 + 128, 0:tw])
            f.dma("sp", x1[i % 3][:, 0:ns, :], X[t0:t0 + tw, :].rearrange("(s p) d -> p s d", p=128))

        def stage_a(i):
            t0, tw, c = tl[i]
            ns = tw // 128
            m_ = mx[i % 2]
            x_ = x1[i % 3]
            for grp in range(4):
                for ci in range(2):
                    f.act(sqt[:, ci, 0:tw], m_[:, 2 * grp + ci, 0:tw], AF.Square)
                ps = g.ps[2 + grp % 2]
                for ci in range(2):
                    f.matmul(ps[:, 0:tw], g.ones[:], sqt[:, ci, 0:tw], start=(ci == 0), stop=(ci == 1))
                rstd_from_sum(g, rg[:, 0:tw], ps[:, 0:tw], 256)
                for ci in range(2):
                    j = 2 * grp + ci
                    f.stt("dve", catT[:, j, 0:tw], m_[:, j, 0:tw], gng[:, j:j + 1], rg[:, 0:tw], ALU.mult, ALU.mult)
            for s in range(ns):
                for half in range(2):
                    ps = g.ps[4 + half]
                    for kc in range(8):
                        f.matmul(ps[:, :], catT[:, kc, s * 128:(s + 1) * 128], Wo[c][:, kc, half * 512:(half + 1) * 512],
                                 start=(kc == 0), stop=(kc == 7))
                    f.tt("dve", x_[:, s, half * 512:(half + 1) * 512], ps[:, :], x_[:, s, half * 512:(half + 1) * 512], ALU.add)

        def stage_b(i):
            t0, tw, c = tl[i]
            ns = tw // 128
            x_ = x1[i % 3]
            for s in range(ns):
                f.act(junk[:], x_[:, s, :], AF.Square, accum_out=ss[:, s:s + 1])
            rstd_from_sum(g, rstd[:, 0:ns], ss[:, 0:ns], 1024)
            for s in range(ns):
                f.act(xn2[:, s, :], x_[:, s, :], AF.Identity, scale=rstd[:, s:s + 1])
            f.act(x_[:, 0:ns, :], x_[:, 0:ns, :], AF.Identity, scale=g.rflag[:, 0:1])
            f.dma("pool", g.X1[t0:t0 + tw, :].rearrange("(s p) d -> p s d", p=128), x_[:, 0:ns, :])
            for s in range(ns):
                f.tt("pool", tmp[:], xn2[:, s, :], A2b[c][:], ALU.mult)
                f.tt("pool", h2row[:, s, 0:1024], tmp[:], B2b[c][:], ALU.add)
                f.memset("pool", h2row[:, s, 1024:1025], float(t0 // 128 + s))
            f.dma("pool", S["H2"][t0:t0 + tw, :].rearrange("(s p) d -> p s d", p=128), h2row[:, 0:ns, :])
            for j in range(8):
                ps = g.ps[j % 2]
                for s in range(ns):
                    f.transpose(ps[:, s * 128:(s + 1) * 128], xn2[:, s, j * 128:(j + 1) * 128], g.ident[:])
                a_ = g.A2[:, l, j, c:c + 1]
                b_ = g.mods[:, l, 3, j, c:c + 1]
                if j % 2:
                    f.act(h2T[:, j, 0:tw], ps[:, 0:tw], AF.Identity, bias=b_, scale=a_)
                else:
                    f.ts("dve", h2T[:, j, 0:tw], ps[:, 0:tw], a_, b_, ALU.mult, ALU.add)
            psl = g.ps[6]
            for s in range(ns):
                for kc in range(8):
                    f.matmul(psl[:, s * 16:(s + 1) * 16], h2T[:, kc, s * 128:(s + 1) * 128], rw[:, kc, :],
                             start=(kc == 0), stop=(kc == 7))
            for s in range(ns):
                f.act(ex[:, s, :], psl[:, s * 16:(s + 1) * 16], AF.Exp, accum_out=se[:, s:s + 1])
            f.recip(se[:, 0:ns], se[:, 0:ns])
            for s in range(ns):
                f.ts("dve", g.AFF[:, t0 // 128 + s, :], ex[:, s, :], se[:, s:s + 1], None, ALU.mult)

        load(0)
        n_t = len(tl)
        for i in range(n_t + 1):
            if i < n_t:
                if i + 1 < n_t:
                    load(i + 1)
                stage_a(i)
            if i >= 1:
                stage_b(i - 1)
        f.dma("sp", S["AFFD"][0:TT, :].rearrange("(t p) e -> p t e", p=128), g.AFF[:])


def route_sets(with_ctx):
    return [(0, 64, CAP_L, 0)] + ([(64, 2, CAP_C, CAP_L)] if with_ctx else [])


def phase_route(g, l, with_ctx, niter=30):
    f, I, S = g.f, g.I, g.S
    with ExitStack() as es:
        sb = lambda n, s, d=F32: g.sb(es, n, s, d)
        lo = sb("lo", [128, 16]); hi = sb("hi", [128, 16]); mid = sb("mid", [128, 16])
        d1 = sb("d1", [128, 16]); d2 = sb("d2", [128, 16]); ge = sb("ge", [128, 16]); part = sb("part", [128, 16])
        cmp = sb("cmp", [128, 16, 64])
        trib = sb("trib", [128, 128], BF16)
        f.dma("pool", trib[:], I["tri"][:, :])
        maskf = sb("maskf", [128, 64, 16])
        maskb = sb("maskb", [128, 64, 16], BF16)
        pos = sb("pos", [128, 64, 16])
        cnt = sb("cnt", [128, 64, 16])
        cum = sb("cum", [128, 16, 64])
        ones64 = sb("ones64", [128, 64])
        f.memset("dve", ones64[:], 1.0)
        eoffi = sb("eoffi", [128, 16], I32)
        eoff = sb("eoff", [128, 16])
        f.op("pool", lambda E: E.iota(eoffi.h[:], pattern=[[CAP_L + CAP_C, 16]], base=0, channel_multiplier=0), [eoffi[:]], [])
        f.copy("dve", eoff[:], eoffi[:])
        for (t0, nt, cap, slot0) in route_sets(with_ctx):
            AFv = g.AFF[:, t0:t0 + nt, :]
            AFe = AFv.rearrange("p t e -> p e t")
            f.memset("dve", lo[:], 0.0)
            f.memset("dve", hi[:], 2.0)
            for it in range(niter):
                f.tt("dve", mid[:], lo[:], hi[:], ALU.add)
                f.ts("dve", mid[:], mid[:], 0.5, None, ALU.mult)
                f.tt("dve", cmp[:, :, 0:nt], AFe, mid[:, :].unsqueeze(2).to_broadcast([128, 16, nt]), ALU.is_ge)
                f.reduce("dve", part[:], cmp[:, :, 0:nt], AX.X, ALU.add)
                ps = g.ps[it % 2]
                f.matmul(ps[:, 0:16], g.ones[:], part[:], start=True, stop=True)
                f.ts("dve", ge[:], ps[:, 0:16], float(cap) - 0.5, None, ALU.is_ge)
                f.tt("dve", d1[:], mid[:], lo[:], ALU.subtract)
                f.tt("dve", d1[:], d1[:], ge[:], ALU.mult)
                f.tt("dve", d2[:], hi[:], mid[:], ALU.subtract)
                f.tt("dve", d2[:], d2[:], ge[:], ALU.mult)
                f.tt("dve", lo[:], lo[:], d1[:], ALU.add)
                f.tt("dve", hi[:], mid[:], d2[:], ALU.add)
            f.tt("dve", maskf[:, 0:nt, :], AFv, lo[:, :].unsqueeze(1).to_broadcast([128, nt, 16]), ALU.is_ge)
            f.copy("dve", maskb[:, 0:nt, :], maskf[:, 0:nt, :])
            n = nt * 16
            mflat = maskb[:, 0:nt, :].rearrange("p t e -> p (t e)")
            pflat = pos[:, 0:nt, :].rearrange("p t e -> p (t e)")
            cflat = cnt[:, 0:nt, :].rearrange("p t e -> p (t e)")
            for q0 in range(0, n, 512):
                w = min(512, n - q0)
                pa = g.ps[2]
                pb = g.ps[3]
                f.matmul(pa[:, 0:w], trib[:], mflat[:, q0:q0 + w], start=True, stop=True)
                f.matmul(pb[:, 0:w], g.onesb[:], mflat[:, q0:q0 + w], start=True, stop=True)
                f.copy("dve", pflat[:, q0:q0 + w], pa[:, 0:w])
                f.copy("act", cflat[:, q0:q0 + w], pb[:, 0:w])
            for e in range(16):
                f.op("dve", lambda E, e=e: E.tensor_tensor_scan(
                    cum.h[:, e, 0:nt], ones64.h[:, 0:nt], cnt.h[:, 0:nt, e], 0.0, ALU.mult, ALU.add),
                    [cum[:, e, 0:nt]], [ones64[:], cnt[:]])
            f.tt("dve", pos[:, 0:nt, :], pos[:, 0:nt, :], cum[:, :, 0:nt].rearrange("p e t -> p t e"), ALU.add)
            f.tt("dve", pos[:, 0:nt, :], pos[:, 0:nt, :], cnt[:, 0:nt, :], ALU.subtract)
            f.ts("dve", cnt[:, 0:nt, :], pos[:, 0:nt, :], float(cap) - 0.5, None, ALU.is_lt)
            f.tt("dve", maskf[:, 0:nt, :], maskf[:, 0:nt, :], cnt[:, 0:nt, :], ALU.mult)
            f.tt("dve", pos[:, 0:nt, :], pos[:, 0:nt, :], eoff[:, :].unsqueeze(1).to_broadcast([128, nt, 16]), ALU.add)
            f.stt("dve", pos[:, 0:nt, :], pos[:, 0:nt, :], float(slot0) - BIG, maskf[:, 0:nt, :], ALU.add, ALU.mult)
            f.ts("dve", pos[:, 0:nt, :], pos[:, 0:nt, :], BIG, None, ALU.add)
            f.copy("dve", g.IDX[:, t0:t0 + nt, :], pos[:, 0:nt, :])
        f.dma("sp", S["DBG"][:, 2048:2048 + NTILE * NEXP].bitcast(I32), g.IDX[:].rearrange("p t e -> p (t e)"))


def dispatch_tiles(g, with_ctx):
    out = []
    for (t0, nt, cap, slot0) in route_sets(with_ctx):
        out += list(range(t0, t0 + nt))
    return out


def dispatch_slice(g, hrow, kctr, experts, tiles):
    f, S = g.f, g.S
    XS = S["XS"]
    for t in tiles:
        h_ = hrow[kctr[0] % len(hrow)]
        kctr[0] += 1
        f.dma("sp", h_[:], S["H2"][t * 128:(t + 1) * 128, :])
        for e in experts:
            f.dma("pool", XS[:, :], h_[:], fn=lambda E, e=e, t=t, h_=h_: E.indirect_dma_start(
                out=XS.h.ap(), out_offset=bass.IndirectOffsetOnAxis(ap=g.IDX.h[:, t, e:e + 1], axis=0),
                in_=h_.h[:], in_offset=None, bounds_check=g.breg, oob_is_err=False))


EGRP = 4


def phase_dispatch(g, l, with_ctx):
    with ExitStack() as es:
        hrow = [g.sb(es, "hrow%d" % i, [128, RW], BF16) for i in range(3)]
        dispatch_slice(g, hrow, [0], list(range(EGRP)), dispatch_tiles(g, with_ctx))


def phase_experts(g, l, with_ctx):
    f, I, S = g.f, g.I, g.S
    X1e = [T(g.X1.h, "X1acc%d" % e, loose=True) for e in range(NEL)]
    with ExitStack() as es:
        sb = lambda n, s, d=F32: g.sb(es, n, s, d)
        ncv = 2 if with_ctx else 1
        gtb = [sb("gt2b%d" % c, [128, 1024]) for c in range(ncv)]
        tmpd = sb("tmpd", [128, 128])
        for c in range(ncv):
            bcast_vec(g, gtb[c], g.mods[:, l, 5, :, c], tmpd, g.ps[6], g.ps[7])
        W = [[sb("W%s%d" % (nm, i), [128, 8, 1024], BF16) for nm in ("g", "u", "d")] for i in range(2)]
        xs = sb("xs", [128, 8, RW], BF16)
        xsc = sb("xsc", [32, RW], BF16)
        xsT = sb("xsT", [128, 8, 1056], BF16)
        hidT = sb("hidT", [128, 8, 1056], BF16)
        sg = [sb("sg%d" % i, [128, 512]) for i in range(2)]
        yrow = [sb("yrow%d" % i, [128, 1024]) for i in range(3)]
        for i in range(3):
            f.memset("dve", yrow[i][:], 0.0)
        ranges = [(0, 512), (512, 512)] + ([(1024, 32)] if with_ctx else [])
        hrow = [sb("hrowx%d" % i, [128, RW], BF16) for i in range(2)]
        hk = [0]
        dtiles = dispatch_tiles(g, with_ctx)

        def loadxs(e):
            f.dma("sp", xs[:], S["XS"][e * 1056:e * 1056 + 1024, :].rearrange("(st p) d -> p st d", p=128))
            if with_ctx:
                f.dma("sp", xsc[:], S["XS"][e * 1056 + 1024:e * 1056 + 1056, :])

        wstg = [sb("wstg%d" % i, [128, 512]) for i in range(8)]

        import os
        XP = os.environ.get("XP", "")

        def w_issue(e, j0, j1):
            if "now" in XP and e > 0:
                return
            for j in range(j0, min(j1, 48)):
                wi, kc, hf = j // 16, (j % 16) // 2, j % 2
                nm = ("wg", "wu", "wd")[wi]
                f.dma("sp", wstg[j % 8][:], I[nm][l, e, kc * 128:(kc + 1) * 128, hf * 512:(hf + 1) * 512])

        def w_cast(e, j0, j1):
            if "now" in XP and e > 0:
                return
            for j in range(j0, min(j1, 48)):
                wi, kc, hf = j // 16, (j % 16) // 2, j % 2
                f.copy("act" if j % 2 else "dve", W[e % 2][wi][:, kc, hf * 512:(hf + 1) * 512], wstg[j % 8][:])

        def loadw(e):
            w_issue(e, 0, 6)
            for i in range(8):
                w_cast(e, 6 * i, 6 * i + 6)
                w_issue(e, 6 * i + 6, 6 * i + 12)

        tokf = [sb("tokf%d" % i, [128, 9]) for i in range(2)]
        toki = [sb("toki%d" % i, [128, 9], I32) for i in range(2)]
        affr = [sb("affr%d" % i, [128, 9, NEXP]) for i in range(2)]
        nst = 9 if with_ctx else 8

        def tok_and_gates(e):
            tf, ti_, af = tokf[e % 2], toki[e % 2], affr[e % 2]
            f.stt("dve", tf[:, 0:8], xs[:, :, 1024], 128.0, xs[:, :, 1025], ALU.mult, ALU.add)
            f.copy("dve", ti_[:, 0:8], tf[:, 0:8])
            if with_ctx:
                f.copy("dve", ti_[:, 8:9], g.dumi[:])
                f.stt("dve", tf[0:32, 8:9], xsc[0:32, 1024:1025], 128.0, xsc[0:32, 1025:1026], ALU.mult, ALU.add)
                f.copy("dve", ti_[0:32, 8:9], tf[0:32, 8:9])
            for st in range(nst):
                f.dma("pool", af[:, st, :], S["AFFD"][:, :], deps=[ti_[:]], fn=lambda E, st=st: E.indirect_dma_start(
                    out=af.h[:, st, :], out_offset=None, in_=S["AFFD"].h.ap(),
                    in_offset=bass.IndirectOffsetOnAxis(ap=ti_.h[:, st:st + 1], axis=0),
                    bounds_check=g.breg2, oob_is_err=False))

        loadw(0)
        loadxs(0)
        tok_and_gates(0)
        k = 0
        yk = 0
        for e in range(NEL):
            Wg, Wu, Wd = W[e % 2]
            ti_, af = toki[e % 2], affr[e % 2]
            pc = g.ps[7][:, :].bitcast(BF16)
            for kc in range(8):
                pb = g.ps[kc % 2][:, :].bitcast(BF16)
                for st in range(8):
                    f.transpose(pb[:, st * 128:(st + 1) * 128], xs[:, st, kc * 128:(kc + 1) * 128], g.identb[:])
                f.evac(xsT[:, kc, 0:1024], pb[:, 0:1024])
                if with_ctx:
                    f.transpose(pc[:, kc * 32:(kc + 1) * 32], xsc[0:32, kc * 128:(kc + 1) * 128], g.identb[0:32, 0:32])
            if with_ctx:
                f.evac(xsT[:, :, 1024:1056], pc[:, 0:256].rearrange("p (k s) -> p k s", k=8))
            nxt_same_grp = (e + 1 < NEL) and ((e + 1) % EGRP != 0)
            if nxt_same_grp:
                loadxs(e + 1)
            if e + 1 < NEL:
                w_issue(e + 1, 0, 6)
            for fc in range(8):
                if e + 1 < NEL:
                    w_cast(e + 1, 6 * fc, 6 * fc + 6)
                    w_issue(e + 1, 6 * fc + 6, 6 * fc + 12)
                for (s0, sw) in ranges:
                    pg = g.ps[2 + (k % 2) * 2]
                    pu = g.ps[3 + (k % 2) * 2]
                    s_ = sg[k % 2]
                    k += 1
                    for kc in range(8):
                        f.matmul(pg[:, 0:sw], Wg[:, kc, fc * 128:(fc + 1) * 128], xsT[:, kc, s0:s0 + sw],
                                 start=(kc == 0), stop=(kc == 7))
                    for kc in range(8):
                        f.matmul(pu[:, 0:sw], Wu[:, kc, fc * 128:(fc + 1) * 128], xsT[:, kc, s0:s0 + sw],
                                 start=(kc == 0), stop=(kc == 7))
                    f.act(s_[:, 0:sw], pg[:, 0:sw], AF.Silu)
                    f.tt("dve", hidT[:, fc, s0:s0 + sw], s_[:, 0:sw], pu[:, 0:sw], ALU.mult)
            tiles = [(st * 128, 128) for st in range(8)] + ([(1024, 32)] if with_ctx else [])
            for ti, (s0, sw) in enumerate(tiles):
                y_ = yrow[yk % 3]
                yk += 1
                c = 1 if ti == 8 else 0
                for half in range(2):
                    py = g.ps[6 + half]
                    for fc in range(8):
                        f.matmul(py[0:sw, :], hidT[:, fc, s0:s0 + sw], Wd[:, fc, half * 512:(half + 1) * 512],
                                 start=(fc == 0), stop=(fc == 7))
                    f.stt("dve", y_[0:sw, half * 512:(half + 1) * 512], py[0:sw, :], af[0:sw, ti, e:e + 1],
                          gtb[c][0:sw, half * 512:(half + 1) * 512], ALU.mult, ALU.mult)
                f.dma("pool", X1e[e][:, :], y_[:], deps=[ti_[:]] + ([X1e[e - 1][:, :]] if e > 0 else []),
                      fn=lambda E, ti=ti, y_=y_, ti_=ti_: E.indirect_dma_start(
                    out=g.X1.h.ap(), out_offset=bass.IndirectOffsetOnAxis(ap=ti_.h[:, ti:ti + 1], axis=0),
                    in_=y_.h[:], in_offset=None, bounds_check=g.breg2, oob_is_err=True, compute_op=ALU.add))
            if nxt_same_grp:
                tok_and_gates(e + 1)
            grp = e // EGRP
            if (grp + 1) * EGRP < NEL:
                j = e % EGRP
                n_ = len(dtiles)
                sl = dtiles[(j * n_) // EGRP:((j + 1) * n_) // EGRP]
                dispatch_slice(g, hrow, hk, list(range((grp + 1) * EGRP, (grp + 2) * EGRP)), sl)
            if e + 1 < NEL and not nxt_same_grp:
                loadxs(e + 1)
                tok_and_gates(e + 1)


def xg_rows(k):
    return 512 if k < 16 else 256


def phase_exchange(g, l, with_ctx):
    f, I, S = g.f, g.I, g.S
    nck = NXCH if with_ctx else 16
    for k in range(nck):
        n = xg_rows(k)
        f.collective(S["XG"][k][:, :], g.X1[:, :], lambda E, k=k, n=n: E.collective_compute(
            "AllGather", ALU.bypass, replica_groups=[[0, 1], [2, 3], [4, 5], [6, 7]],
            ins=[g.X1.h[k * 512:k * 512 + n, :].opt()], outs=[S["XG"][k].h.ap().opt()]))
    if True:
        return
    with ExitStack() as es:
        a = [g.sb(es, "xa%d" % i, [128, 4, 1024]) for i in range(2)]
        b = [g.sb(es, "xb%d" % i, [128, 4, 1024]) for i in range(2)]
        for k in range(nck):
            n = xg_rows(k)
            ns = n // 128
            a_, b_ = a[k % 2], b[k % 2]
            f.dma("sp", a_[:, 0:ns, :], S["XG"][k][0:n, :].rearrange("(s p) d -> p s d", p=128))
            f.dma("sp", b_[:, 0:ns, :], S["XG"][k][n:2 * n, :].rearrange("(s p) d -> p s d", p=128))
            f.tt("dve" if k % 2 else "pool", a_[:, 0:ns, :], a_[:, 0:ns, :], b_[:, 0:ns, :], ALU.add)
            f.dma("sp", S["X2"][k * 512:k * 512 + n, :].rearrange("(s p) d -> p s d", p=128), a_[:, 0:ns, :])


def phase_final(g):
    f, I, S = g.f, g.I, g.S
    with ExitStack() as es:
        sb = lambda n, s, d=F32: g.sb(es, n, s, d)
        fnb = sb("fnb", [128, 1024])
        f.dma("sp", fnb[:], I["fnorm"][:].partition_broadcast(128))
        xt = [sb("xf%d" % i, [128, 4, 1024]) for i in range(2)]
        xu = [sb("xu%d" % i, [128, 4, 1024]) for i in range(2)]
        junk = sb("junkf", [128, 1024], BF16)
        ss = sb("ssf", [128, 4])
        rstd = sb("rstdf", [128, 4])
        nt = NL // 512

        def load(i):
            f.dma("sp", xt[i % 2][:], S["XG"][i][0:512, :].rearrange("(s p) d -> p s d", p=128))
            f.dma("sp", xu[i % 2][:], S["XG"][i][512:1024, :].rearrange("(s p) d -> p s d", p=128))

        load(0)
        for i in range(nt):
            if i + 1 < nt:
                load(i + 1)
            x_ = xt[i % 2]
            f.tt("dve", x_[:], x_[:], xu[i % 2][:], ALU.add)
            for s in range(4):
                f.act(junk[:], x_[:, s, :], AF.Square, accum_out=ss[:, s:s + 1])
            rstd_from_sum(g, rstd[:], ss[:], 1024)
            for s in range(4):
                f.act(x_[:, s, :], x_[:, s, :], AF.Identity, scale=rstd[:, s:s + 1])
                f.tt("dve" if s % 2 else "pool", x_[:, s, :], x_[:, s, :], fnb[:], ALU.mult)
            f.dma("sp", g.OUT[i * 512:(i + 1) * 512, :].rearrange("(s p) d -> p s d", p=128), x_[:])


_NC_CACHE = {}


def kernel(**inputs):
    inp = {k: np.asarray(v) for k, v in inputs.items()}
    B = inp["x"].shape[0]
    if "nc" not in _NC_CACHE:
        _NC_CACHE["nc"] = build()
    nc = _NC_CACHE["nc"]
    in_maps = [host_prep(inp, c // 2, c % 2) for c in range(2 * B)]
    res = run_bass_kernel_spmd(nc, in_maps, core_ids=list(range(2 * B)))
    out = np.stack([np.asarray(res.results[2 * b]["out"], dtype=np.float32) for b in range(B)], axis=0)
    return out
```

```python
import math
from contextlib import ExitStack
import numpy as np
import concourse.bass as bass
import concourse.mybir as mybir
from concourse.bass_utils import run_bass_kernel_spmd

F32 = mybir.dt.float32
BF16 = mybir.dt.bfloat16
I32 = mybir.dt.int32
AF = mybir.ActivationFunctionType
ALU = mybir.AluOpType
AX = mybir.AxisListType

NL = 8192
NC_ = 256
TT = NL + NC_
D = 1024
NTILE = TT // 128
EPS = 1e-6
NEXP = 16
NEL = 8
NXCH = 17
CAP_L = 1024
CAP_C = 32
RW = 1028
BIG = 1.0e6
SAME_ENGINE_NOWAIT = ("pe",)


class T:
    __slots__ = ("h", "writes", "reads", "name", "loose")

    def __init__(self, h, name="", loose=False):
        self.h = h
        self.writes = {}
        self.reads = {}
        self.name = name
        self.loose = loose

    def __getitem__(self, k):
        return V(self, self.h[k])


class V:
    __slots__ = ("t", "ap")

    def __init__(self, t, ap):
        self.t = t
        self.ap = ap

    def __getitem__(self, k):
        return V(self.t, self.ap[k])

    def rearrange(self, s, **kw):
        return V(self.t, self.ap.rearrange(s, **kw))

    def bitcast(self, dt):
        return V(self.t, self.ap.bitcast(dt))

    def to_broadcast(self, shape):
        return V(self.t, self.ap.to_broadcast(shape))

    def unsqueeze(self, a):
        return V(self.t, self.ap.unsqueeze(a))

    def partition_broadcast(self, n):
        return V(self.t, self.ap.partition_broadcast(n))


def _ap(x):
    return x.ap if isinstance(x, V) else x


class FW:
    ENGS = ("pe", "act", "dve", "pool", "sp")

    def __init__(self, nc, ndma=8):
        self.nc = nc
        self.eng = {"pe": nc.tensor, "act": nc.scalar, "dve": nc.vector, "pool": nc.gpsimd, "sp": nc.sync}
        self.sem = {}
        self.cnt = {}
        for e in ("pe", "act", "dve", "pool"):
            self.sem[e] = nc.alloc_semaphore("S_" + e)
            self.cnt[e] = 0
        self.dring = {}
        for q in ("sp", "pool"):
            ring = []
            for i in range(ndma):
                key = "D_%s_%d" % (q, i)
                self.sem[key] = nc.alloc_semaphore(key)
                self.cnt[key] = 0
                ring.append(key)
            self.dring[q] = [ring, 0]
        self.sem["cc"] = nc.alloc_semaphore("S_cc")
        self.cnt["cc"] = 0
        self.known = {e: {} for e in self.ENGS}
        self.ninstr = 0
        self._ev = 0

    def _wait(self, e, k, v):
        if e == k and e in SAME_ENGINE_NOWAIT:
            return
        kn = self.known[e]
        if kn.get(k, 0) >= v:
            return
        kn[k] = v
        self.eng[e].wait_ge(self.sem[k], v)
        self.ninstr += 1

    def _deps(self, e, ins, outs):
        for x in ins:
            if isinstance(x, V):
                for k, v in x.t.writes.items():
                    self._wait(e, k, v)
        for x in outs:
            t = x.t
            if t.loose:
                continue
            for k, v in t.writes.items():
                self._wait(e, k, v)
            for k, v in t.reads.items():
                self._wait(e, k, v)

    def _done(self, k, v, ins, outs):
        for x in ins:
            if isinstance(x, V):
                r = x.t.reads
                if r.get(k, 0) < v:
                    r[k] = v
        for x in outs:
            t = x.t
            if t.loose:
                if t.writes.get(k, 0) < v:
                    t.writes[k] = v
            else:
                t.writes = {k: v}
                t.reads = {}

    def op(self, e, fn, outs, ins):
        self._deps(e, ins, outs)
        inst = fn(self.eng[e])
        self.cnt[e] += 1
        inst.then_inc(self.sem[e], 1)
        self.ninstr += 1
        self._done(e, self.cnt[e], ins, outs)
        return inst

    def dma(self, q, out, in_, fn=None, deps=(), **kw):
        ring, pos = self.dring[q]
        key = ring[pos % len(ring)]
        self.dring[q][1] = pos + 1
        if self.cnt[key] > 0:
            self._wait(q, key, self.cnt[key])
        self._deps(q, [in_] + list(deps), [out])
        if fn is None:
            inst = self.eng[q].dma_start(out=_ap(out), in_=_ap(in_), **kw)
        else:
            inst = fn(self.eng[q])
        self.cnt[key] += 16
        inst.then_inc(self.sem[key], 16)
        self.ninstr += 1
        self._done(key, self.cnt[key], [in_] + list(deps), [out])
        return inst

    def collective(self, out, in_, fn):
        q = "pool"
        if self.cnt["cc"] > 0:
            self._wait(q, "cc", self.cnt["cc"])
        self._deps(q, [in_], [out])
        inst = fn(self.eng[q])
        self.cnt["cc"] += 1
        inst.then_inc(self.sem["cc"], 1)
        self.ninstr += 1
        self._done("cc", self.cnt["cc"], [in_], [out])
        return inst

    def barrier(self):
        toks = [(k, v) for k, v in self.cnt.items() if v > 0]
        for e in self.ENGS:
            for k, v in toks:
                self._wait(e, k, v)

    def matmul(self, out, lhsT, rhs, start=True, stop=True):
        return self.op("pe", lambda E: E.matmul(_ap(out), _ap(lhsT), _ap(rhs), start=start, stop=stop),
                       [out], [lhsT, rhs])

    def transpose(self, out, in_, ident):
        return self.op("pe", lambda E: E.transpose(_ap(out), _ap(in_), _ap(ident)), [out], [in_, ident])

    def act(self, out, in_, func, bias=None, scale=None, accum_out=None):
        kw = {}
        ins = [in_]
        outs = [out]
        if bias is not None:
            kw["bias"] = _ap(bias)
            ins.append(bias)
        if scale is not None:
            kw["scale"] = _ap(scale)
            ins.append(scale)
        if accum_out is not None:
            kw["accum_out"] = _ap(accum_out)
            outs.append(accum_out)
        return self.op("act", lambda E: E.activation(_ap(out), _ap(in_), func, **kw), outs, ins)

    def tt(self, e, out, in0, in1, op):
        return self.op(e, lambda E: E.tensor_tensor(_ap(out), _ap(in0), _ap(in1), op), [out], [in0, in1])

    def ts(self, e, out, in0, s1, s2, op0, op1=None, accum_out=None):
        kw = {}
        outs = [out]
        if op1 is not None:
            kw["op1"] = op1
        if accum_out is not None:
            kw["accum_out"] = _ap(accum_out)
            outs.append(accum_out)
        return self.op(e, lambda E: E.tensor_scalar(_ap(out), _ap(in0), _ap(s1), _ap(s2), op0, **kw),
                       outs, [in0, s1, s2])

    def stt(self, e, out, in0, scalar, in1, op0, op1):
        return self.op(e, lambda E: E.scalar_tensor_tensor(_ap(out), _ap(in0), _ap(scalar), _ap(in1), op0, op1),
                       [out], [in0, scalar, in1])

    def copy(self, e, out, in_):
        if e == "act":
            return self.op(e, lambda E: E.copy(_ap(out), _ap(in_)), [out], [in_])
        return self.op(e, lambda E: E.tensor_copy(_ap(out), _ap(in_)), [out], [in_])

    def evac(self, out, in_):
        self._ev += 1
        return self.copy("act" if self._ev % 2 else "dve", out, in_)

    def memset(self, e, out, val):
        return self.op(e, lambda E: E.memset(_ap(out), val), [out], [])

    def recip(self, out, in_):
        return self.op("dve", lambda E: E.reciprocal(_ap(out), _ap(in_)), [out], [in_])

    def reduce(self, e, out, in_, axis, op):
        return self.op(e, lambda E: E.tensor_reduce(_ap(out), _ap(in_), axis, op), [out], [in_])


def _consts():
    c = {}
    c["ident"] = np.eye(128, dtype=np.float32)
    t = np.arange(NL)
    rows = (t // 64).astype(np.float64)
    cols = (t % 64).astype(np.float64)
    inv = 10000.0 ** (-np.arange(8, dtype=np.float64) / 8)
    rc = np.zeros((32, NL), np.float64)
    rs = np.zeros((32, NL), np.float64)
    for i in range(8):
        for base, pos in ((0, rows), (16, cols)):
            ang = pos * inv[i]
            rc[base + i] = np.cos(ang); rs[base + i] = -np.sin(ang)
            rc[base + 8 + i] = np.cos(ang); rs[base + 8 + i] = np.sin(ang)
    c["ropeC"] = rc.astype(np.float32)
    c["ropeS"] = rs.astype(np.float32)
    k = np.arange(128, dtype=np.float64)
    a = 2 * np.pi * np.outer(k, k) / 128
    c["c128"] = np.cos(a).astype(np.float32)
    c["s128"] = np.sin(a).astype(np.float32)
    k64 = np.arange(64, dtype=np.float64)
    a64 = 2 * np.pi * np.outer(k64, k64) / 64
    bdc = np.zeros((2, 128, 128), np.float32)
    bds = np.zeros((2, 128, 128), np.float32)
    for ch in range(2):
        for g in range(2):
            bdc[ch, g * 64:(g + 1) * 64, g * 64:(g + 1) * 64] = np.cos(a64)
            bds[ch, g * 64:(g + 1) * 64, g * 64:(g + 1) * 64] = np.sin(a64)
    c["bdc64"] = bdc
    c["bds64"] = bds
    n2 = np.arange(64, dtype=np.float64)[:, None, None]
    k1 = np.arange(128, dtype=np.float64)[None, :, None]
    k2 = np.arange(64, dtype=np.float64)[None, None, :]
    ang = 2 * np.pi * n2 * (k1 + 128 * k2) / 8192
    c["c2"] = np.cos(ang).astype(np.float32)
    c["s2"] = np.sin(ang).astype(np.float32)
    k256 = np.arange(256, dtype=np.float64)
    a256 = 2 * np.pi * np.outer(k256, k256) / 256
    c["c256"] = np.cos(a256).astype(np.float32)
    c["s256"] = np.sin(a256).astype(np.float32)
    c["tri"] = (np.arange(128)[:, None] < np.arange(128)[None, :]).astype(np.float32)
    sel = np.zeros((128, 64), np.float32)
    sel[64 + np.arange(64), np.arange(64)] = 1.0
    c["sel"] = sel
    pc = np.ones((2, 128, 16), np.float32)
    for ch in range(2):
        for g in range(2):
            w = (2, 4, 8, 16)[ch * 2 + g]
            for j in range(8):
                cnt = (j + w - w // 2) - max(j - w // 2, 0)
                pc[ch, g * 64:(g + 1) * 64, j] = w / cnt
                cnt2 = min(w - w // 2, 8 - j) + w // 2
                pc[ch, g * 64:(g + 1) * 64, 8 + j] = w / cnt2
    c["poolcorr"] = pc
    ms = np.zeros((5, 128, 5, 128), np.float32)
    dr_i = np.zeros((5, 128, 5, 128), np.int64)
    dc_i = np.zeros((5, 128, 5, 128), np.int64)
    kl = np.arange(128)
    for v, m in enumerate((0, 1, 2, 62, 63)):
        p0 = min(max(m - 2, 0), 59)
        for ch in range(5):
            krow = 2 * (p0 + ch) + kl // 64
            kcol = kl % 64
            qrow = 2 * m + kl // 64
            qcol = kl % 64
            rs_ = np.clip(qrow - 4, 0, 120)
            cs_ = np.clip(qcol - 8, 0, 48)
            okr = (krow[:, None] >= rs_[None, :]) & (krow[:, None] < rs_[None, :] + 8)
            okc = (kcol[:, None] >= cs_[None, :]) & (kcol[:, None] < cs_[None, :] + 16)
            ok = okr & okc
            dr = krow[:, None] - qrow[None, :] + 7
            dc = kcol[:, None] - qcol[None, :] + 15
            ms[v, :, ch, :] = np.where(ok, 0.0, -30000.0)
            dr_i[v, :, ch, :] = np.where(ok, dr, 0)
            dc_i[v, :, ch, :] = np.where(ok, dc, 0)
    c["namask"] = ms
    c["_dr"] = dr_i
    c["_dc"] = dc_i
    return c


_CONST = None


def _get_consts():
    global _CONST
    if _CONST is None:
        _CONST = _consts()
    return _CONST


CONST_NAMES = ("ident", "ropeC", "ropeS", "c128", "s128", "bdc64", "bds64", "c2", "s2", "c256", "s256",
               "tri", "sel", "poolcorr", "namask")


def _fm(v, nchunk):
    s = v.shape[:-1]
    return np.ascontiguousarray(np.swapaxes(v.reshape(*s, nchunk, 128), -1, -2))


def host_prep(inp, b, r=0):
    C = _get_consts()
    m = {}
    m["xin"] = np.ascontiguousarray(np.concatenate([inp["x"][b], inp["ctx"][b]], axis=0))
    cc = np.stack([inp["c"][b], inp["c_ctx"]], axis=-1)
    m["cc"] = np.ascontiguousarray(cc.reshape(8, 128, 2).transpose(1, 0, 2))
    m["ada_w"] = inp["ada_w"]
    m["ada_bT"] = _fm(inp["ada_b"], 48)
    m["n1g"] = _fm(inp["norm1_g"], 8)
    m["n2g"] = _fm(inp["norm2_g"], 8)
    m["fnorm"] = inp["final_norm"]
    w_in = inp["w_in"]
    if r == 1:
        perm = np.arange(w_in.shape[-1])
        for c0 in (0, 256, 512):
            perm[c0:c0 + 128], perm[c0 + 128:c0 + 256] = np.arange(c0 + 128, c0 + 256), np.arange(c0, c0 + 128)
        w_in = w_in[:, :, perm]
    m["w_in"] = w_in
    nb = inp["na_bias"][:, 2 * r:2 * r + 2]
    m["nab"] = np.ascontiguousarray(
        nb[:, :, C["_dr"], C["_dc"]].transpose(0, 2, 3, 1, 4, 5))
    pw = inp["pool_w"]
    bd = np.zeros((2, 2, 128, 128), np.float32)
    for l in range(2):
        for g in range(4):
            bd[l, g // 2, (g % 2) * 64:(g % 2 + 1) * 64, (g % 2) * 64:(g % 2 + 1) * 64] = pw[l, g]
    m["pool_wbd"] = bd
    m["pool_scT"] = _fm(inp["pool_scale"], 2)
    m["fno_w"] = inp["fno_w"]
    m["qng"] = _fm(inp["mla_q_norm"], 2)
    m["kvg"] = _fm(inp["mla_kv_norm"], 1)
    m["w_uq"] = inp["mla_w_uq"][:, :, 192 * r:192 * r + 192]
    m["w_uk"] = inp["mla_w_uk"][:, :, 128 * r:128 * r + 128]
    m["w_uv"] = inp["mla_w_uv"][:, :, 128 * r:128 * r + 128]
    m["gng"] = _fm(inp["grp_norm"].reshape(2, 1024), 8)
    m["w_out"] = inp["w_out"]
    eperm = list(range(8 * r, 8 * r + 8)) + list(range(8 * (1 - r), 8 * (1 - r) + 8))
    m["router_w"] = inp["router_w"][:, :, eperm]
    m["wg"] = inp["exp_w_gate"][:, 8 * r:8 * r + 8]
    m["wu"] = inp["exp_w_up"][:, 8 * r:8 * r + 8]
    m["wd"] = inp["exp_w_down"][:, 8 * r:8 * r + 8]
    m["rflag"] = np.full((128, 1), 1.0 if r == 0 else 0.0, np.float32)
    for k in CONST_NAMES:
        m[k] = C[k]
    return {k: np.ascontiguousarray(v, dtype=np.float32) for k, v in m.items()}


INPUT_SHAPES = {
    "xin": [TT, D], "cc": [128, 8, 2], "ada_w": [2, D, 6 * D], "ada_bT": [2, 128, 48], "n1g": [2, 128, 8],
    "n2g": [2, 128, 8], "fnorm": [D], "w_in": [2, D, 1696], "nab": [2, 5, 128, 2, 5, 128],
    "pool_wbd": [2, 2, 128, 128], "pool_scT": [2, 128, 2], "fno_w": [2, 256, 256], "qng": [2, 128, 2],
    "kvg": [2, 128, 1], "w_uq": [2, 256, 192], "w_uk": [2, 128, 128], "w_uv": [2, 128, 128], "gng": [2, 128, 8],
    "w_out": [2, D, D], "router_w": [2, D, 16], "wg": [2, 8, D, D], "wu": [2, 8, D, D], "wd": [2, 8, D, D], "rflag": [128, 1],
    "ident": [128, 128], "ropeC": [32, NL], "ropeS": [32, NL], "c128": [128, 128], "s128": [128, 128],
    "bdc64": [2, 128, 128], "bds64": [2, 128, 128], "c2": [64, 128, 64], "s2": [64, 128, 64],
    "c256": [256, 256], "s256": [256, 256], "tri": [128, 128], "sel": [128, 64], "poolcorr": [2, 128, 16],
    "namask": [5, 128, 5, 128],
}


class Ctx:
    pass


def build(stop=None, debug=(), layers=2):
    nc = bass.Bass("TRN2", target_bir_lowering=False)
    f = FW(nc)
    g = Ctx()
    g.nc, g.f = nc, f
    uid = [0]

    def dram(name, shape, dt=F32, loose=True):
        kind = "ExternalOutput" if name in debug else "Internal"
        return T(nc.dram_tensor(name, list(shape), dt, kind=kind), name, loose=loose)

    def sb(es, name, shape, dt=F32):
        uid[0] += 1
        return T(es.enter_context(nc.sbuf_tensor("%s_%d" % (name, uid[0]), list(shape), dt)), name)

    g.dram, g.sb = dram, sb
    I = {k: T(nc.dram_tensor(k, v, F32, kind="ExternalInput"), k) for k, v in INPUT_SHAPES.items()}
    g.I = I
    OUT = T(nc.dram_tensor("out", [NL, D], F32, kind="ExternalOutput"), "out", loose=True)
    g.OUT = OUT
    psb = [nc.alloc_psum_tensor("psb%d" % j, [128, 1024], F32) for j in range(4)]
    g.ps = [T(psb[i // 2][:, (i % 2) * 512:(i % 2 + 1) * 512], "ps%d" % i) for i in range(8)]
    g.ps2 = [T(psb[j][:, :], "ps2_%d" % j) for j in range(4)]

    S = {}
    S["QAT"] = dram("QAT", [128, TT], BF16)
    S["KAT"] = dram("KAT", [128, TT], BF16)
    S["VA"] = dram("VA", [TT, 2, 66], BF16)
    S["MAo"] = [dram("MAo%d" % k, [n * 128, 512], F32) for k, n in enumerate((8, 8, 1))]
    S["MAa"] = [dram("MAa%d" % k, [2 * n * 128, 512], F32) for k, n in enumerate((8, 8, 1))]
    S["PLT"] = dram("PLT", [256, TT], F32)
    S["FLT"] = dram("FLT", [256, TT], BF16)
    S["QT"] = dram("QT", [2, 96, TT], BF16)
    S["KT"] = dram("KT", [2, 96, TT], BF16)
    S["VD"] = dram("VD", [TT, 2, 128], BF16)
    S["MDo"] = [dram("MDo%d" % k, [n * 128, 512], F32) for k, n in enumerate((8, 8, 1))]
    S["MDa"] = [dram("MDa%d" % k, [2 * n * 128, 512], F32) for k, n in enumerate((8, 8, 1))]
    S["MIXT"] = dram("MIXT", [D, TT], F32)
    S["X1a"] = dram("X1a", [TT + 128, D], F32)
    S["X1b"] = dram("X1b", [TT + 128, D], F32)
    S["H2"] = dram("H2", [TT, RW], BF16)
    S["XS"] = dram("XS", [NEL * (CAP_L + CAP_C), RW], BF16)
    S["XG"] = [dram("XG%d" % k, [1024 if k < 16 else 512, D], F32) for k in range(NXCH)]
    S["X2"] = dram("X2", [TT, D], F32)
    S["AFFD"] = dram("AFFD", [TT + 128, NEXP], F32)
    S["BR"] = dram("BR", [128, 64, 256], BF16)
    S["BI"] = dram("BI", [128, 64, 256], BF16)
    S["DBG"] = dram("DBG", [128, 4096], F32)
    g.S = S

    with ExitStack() as ges:
        g.ident = sb(ges, "ident", [128, 128])
        g.identb = sb(ges, "identb", [128, 128], BF16)
        g.ones = sb(ges, "ones", [128, 128])
        g.onesb = sb(ges, "onesb", [128, 128], BF16)
        g.eps = sb(ges, "eps", [128, 1])
        g.mods = sb(ges, "mods", [128, 2, 6, 8, 2])
        g.A1 = sb(ges, "A1", [128, 2, 8, 2])
        g.A2 = sb(ges, "A2", [128, 2, 8, 2])
        g.AFF = sb(ges, "AFF", [128, NTILE, NEXP])
        g.IDX = sb(ges, "IDX", [128, NTILE, NEXP], I32)
        f.dma("sp", g.ident[:], I["ident"][:, :])
        f.copy("dve", g.identb[:], g.ident[:])
        f.memset("dve", g.ones[:], 1.0)
        f.memset("dve", g.onesb[:], 1.0)
        f.memset("dve", g.eps[:], EPS)

        g.breg = nc.gpsimd.to_reg(NEL * (CAP_L + CAP_C) - 1)
        g.breg2 = nc.gpsimd.to_reg(TT + 127)
        g.pidxi = sb(ges, "pidxi", [128, 1], I32)
        g.pidxb = sb(ges, "pidxb", [128, 1], BF16)
        g.dumi = sb(ges, "dumi", [128, 1], I32)
        g.rflag = sb(ges, "rflag", [128, 1])
        f.dma("sp", g.rflag[:], I["rflag"][:, :])
        f.op("pool", lambda E: E.iota(g.pidxi.h[:], pattern=[[0, 1]], base=0, channel_multiplier=1), [g.pidxi[:]], [])
        f.op("pool", lambda E: E.iota(g.dumi.h[:], pattern=[[0, 1]], base=TT, channel_multiplier=1), [g.dumi[:]], [])
        f.copy("dve", g.pidxb[:], g.pidxi[:])
        phase_adaln(g)
        f.barrier()
        if stop == "adaln":
            return finish(g)
        XCUR = I["xin"]
        for l in range(layers):
            last = (l == 1)
            phase_a(g, l, XCUR, from_xg=(l > 0))
            f.barrier()
            if stop == "a%d" % l:
                return finish(g)
            phase_na(g, l, not last)
            f.barrier()
            if stop == "na%d" % l:
                return finish(g)
            phase_pool(g, l, not last)
            f.barrier()
            if stop == "pool%d" % l:
                return finish(g)
            phase_fft(g, l, not last)
            f.barrier()
            if stop == "fft%d" % l:
                return finish(g)
            phase_mla(g, l, not last)
            f.barrier()
            if stop == "mla%d" % l:
                return finish(g)
            g.X1 = S["X1a"] if l == 0 else S["X1b"]
            phase_merge(g, l, XCUR, not last)
            f.barrier()
            if stop == "merge%d" % l:
                return finish(g)
            phase_route(g, l, not last)
            f.barrier()
            if stop == "route%d" % l:
                return finish(g)
            phase_dispatch(g, l, not last)
            f.barrier()
            if stop == "disp%d" % l:
                return finish(g)
            phase_experts(g, l, not last)
            f.barrier()
            if stop == "exp%d" % l:
                return finish(g)
            phase_exchange(g, l, not last)
            f.barrier()
            XCUR = S["X2"]
        phase_final(g)
        return finish(g)


def finish(g):
    g.f.barrier()
    return g.nc


def rstd_from_sum(g, out, in_, n, e_recip="dve"):
    f = g.f
    npart = _ap(out).shape[0]
    f.act(out, in_, AF.Sqrt, bias=g.eps[0:npart, :], scale=1.0 / n)
    f.recip(out, out)


def phase_adaln(g):
    f, I = g.f, g.I
    with ExitStack() as es:
        cc = g.sb(es, "cc", [128, 8, 2])
        sc = g.sb(es, "sc", [128, 8, 2])
        adab = g.sb(es, "adab", [128, 2, 48])
        n1g = g.sb(es, "n1g", [128, 2, 8])
        n2g = g.sb(es, "n2g", [128, 2, 8])
        aw = [g.sb(es, "aw%d" % i, [128, 8, 1024]) for i in range(2)]
        f.dma("sp", cc[:], I["cc"][:])
        for l in range(2):
            f.dma("sp", adab[:, l, :], I["ada_bT"][l])
            f.dma("sp", n1g[:, l, :], I["n1g"][l])
            f.dma("sp", n2g[:, l, :], I["n2g"][l])
        f.act(sc[:], cc[:], AF.Silu)
        k = 0
        for l in range(2):
            for grp in range(6):
                a = aw[k % 2]
                k += 1
                f.dma("sp", a[:], I["ada_w"][l, :, grp * 1024:(grp + 1) * 1024].rearrange("(j p) n -> p j n", p=128))
                ps = g.ps[k % 2]
                for j in range(8):
                    for kc in range(8):
                        f.matmul(ps[:, 2 * j:2 * j + 2], a[:, kc, j * 128:(j + 1) * 128], sc[:, kc, :],
                                 start=(kc == 0), stop=(kc == 7))
                for s in range(2):
                    f.tt("dve", g.mods[:, l, grp, :, s], ps[:, s:16:2], adab[:, l, grp * 8:(grp + 1) * 8], ALU.add)
            for s in range(2):
                f.stt("dve", g.A1[:, l, :, s], g.mods[:, l, 1, :, s], 1.0, n1g[:, l, :], ALU.add, ALU.mult)
                f.stt("dve", g.A2[:, l, :, s], g.mods[:, l, 4, :, s], 1.0, n2g[:, l, :], ALU.add, ALU.mult)


def bcast_vec(g, dst, vec8, tmp, psA, psB):
    f = g.f
    for j in range(8):
        f.ts("dve", tmp[:], g.ident[:], vec8[:, j:j + 1], None, ALU.mult)
        ps = psA if j < 4 else psB
        jj = j % 4
        f.matmul(ps[:, jj * 128:(jj + 1) * 128], g.ones[:], tmp[:], start=True, stop=True)
        if jj == 3:
            f.evac(dst[:, (j - 3) * 128:(j + 1) * 128], ps[:, :])


def tiles512():
    out = [(i * 512, 512, 0) for i in range(16)]
    out.append((NL, 256, 1))
    return out


def phase_a(g, l, X, from_xg=False):
    f, I, S = g.f, g.I, g.S
    with ExitStack() as es:
        sb = lambda n, s, d=F32: g.sb(es, n, s, d)
        win = sb("win", [128, 8, 1696], BF16)
        wkr = sb("wkr", [128, 8, 96], BF16)
        wkrs = sb("wkrs", [128, 8, 96], BF16)
        wuq = sb("wuq", [128, 2, 192], BF16)
        wuqs = sb("wuqs", [128, 2, 2, 96], BF16)
        wuk = sb("wuk", [128, 128], BF16)
        wuv = sb("wuv", [128, 128], BF16)
        qng = sb("qng", [128, 2])
        kvg = sb("kvg", [128, 1])
        for j in range(8):
            f.dma("pool", win[:, j, :], I["w_in"][l, j * 128:(j + 1) * 128, :])
        f.dma("pool", wuq[:], I["w_uq"][l].rearrange("(j p) n -> p j n", p=128))
        f.dma("pool", wuk[:], I["w_uk"][l])
        f.dma("pool", wuv[:], I["w_uv"][l])
        f.dma("sp", qng[:], I["qng"][l])
        f.dma("sp", kvg[:], I["kvg"][l])
        f.memset("dve", wkr[:], 0.0)
        f.memset("dve", wkrs[:], 0.0)
        f.memset("dve", wuqs[:], 0.0)
        f.copy("dve", wkr[:, :, 64:96], win[:, :, 1664:1696])
        for (d0, s0) in ((64, 1672), (72, 1664), (80, 1688), (88, 1680)):
            f.copy("dve", wkrs[:, :, d0:d0 + 8], win[:, :, s0:s0 + 8])
        for h in range(2):
            b0 = 96 * h + 64
            for (d0, s0) in ((64, b0 + 8), (72, b0), (80, b0 + 24), (88, b0 + 16)):
                f.copy("dve", wuqs[:, :, h, d0:d0 + 8], wuq[:, :, s0:s0 + 8])

        xt = [sb("xt%d" % i, [128, 4, 1024]) for i in range(2)]
        xu = [sb("xu%d" % i, [128, 4, 1024]) for i in range(2)] if from_xg else None
        xn = sb("xn", [128, 4, 1024])
        junk = sb("junk", [128, 1024], BF16)
        ss = sb("ss", [128, 4])
        rstd = sb("rstd", [128, 4])
        hT = [sb("hT%d" % i, [128, 8, 512], BF16) for i in range(2)]
        stg = [sb("stg%d" % i, [128, 4, 512], BF16) for i in range(2)]
        plst = [sb("plst%d" % i, [128, 2, 512]) for i in range(2)]
        qc = sb("qc", [128, 2, 512])
        kvc = sb("kvc", [128, 512])
        sq = sb("sq", [128, 3, 512])
        rq = sb("rq", [128, 512])
        rk = sb("rk", [128, 512])
        qn = sb("qn", [128, 2, 512], BF16)
        ckv = sb("ckv", [128, 512], BF16)
        rc = [sb("rc%d" % i, [96, 512]) for i in range(2)]
        rs = [sb("rs%d" % i, [96, 512]) for i in range(2)]
        t1 = sb("t1", [96, 512])
        t2 = sb("t2", [96, 512])
        qst = [sb("qst%d" % i, [96, 2, 512], BF16) for i in range(2)]
        kst = [sb("kst%d" % i, [96, 2, 512], BF16) for i in range(2)]
        vast = [sb("vast%d" % i, [128, 4, 2, 66], BF16) for i in range(2)]
        vdst = [sb("vdst%d" % i, [128, 4, 2, 128], BF16) for i in range(2)]
        for i in range(2):
            f.memset("pool", vast[i][:], 1.0)
            f.memset("pool", vdst[i][:], 1.0)
        rot = [2]

        def nxt():
            p = g.ps[rot[0]]
            rot[0] = 2 + (rot[0] - 2 + 1) % 6
            return p

        tl = tiles512()

        def load(i):
            t0, tw, c = tl[i]
            ns = tw // 128
            if from_xg:
                f.dma("sp", xt[i % 2][:, 0:ns, :], S["XG"][i][0:tw, :].rearrange("(s p) d -> p s d", p=128))
                f.dma("sp", xu[i % 2][:, 0:ns, :], S["XG"][i][tw:2 * tw, :].rearrange("(s p) d -> p s d", p=128))
            else:
                f.dma("sp", xt[i % 2][:, 0:ns, :], X[t0:t0 + tw, :].rearrange("(s p) d -> p s d", p=128))
            if c == 0:
                f.dma("sp", rc[i % 2][64:96, :], I["ropeC"][:, t0:t0 + 512])
                f.dma("sp", rs[i % 2][64:96, :], I["ropeS"][:, t0:t0 + 512])

        load(0)
        for i, (t0, tw, c) in enumerate(tl):
            ns = tw // 128
            if i + 1 < len(tl):
                load(i + 1)
            x_ = xt[i % 2]
            h_ = hT[i % 2]
            st_ = stg[i % 2]
            pl_ = plst[i % 2]
            if from_xg:
                f.tt("dve", x_[:, 0:ns, :], x_[:, 0:ns, :], xu[i % 2][:, 0:ns, :], ALU.add)
                f.dma("pool", S["X2"][t0:t0 + tw, :].rearrange("(s p) d -> p s d", p=128), x_[:, 0:ns, :])
            for s in range(ns):
                f.act(junk[:], x_[:, s, :], AF.Square, accum_out=ss[:, s:s + 1])
            rstd_from_sum(g, rstd[:, 0:ns], ss[:, 0:ns], 1024)
            for s in range(ns):
                if s % 2:
                    f.act(xn[:, s, :], x_[:, s, :], AF.Identity, scale=rstd[:, s:s + 1])
                else:
                    f.ts("dve", xn[:, s, :], x_[:, s, :], rstd[:, s:s + 1], None, ALU.mult)
            for j in range(8):
                ps = g.ps[j % 2]
                for s in range(ns):
                    f.transpose(ps[:, s * 128:(s + 1) * 128], xn[:, s, j * 128:(j + 1) * 128], g.ident[:])
                a_ = g.A1[:, l, j, c:c + 1]
                b_ = g.mods[:, l, 0, j, c:c + 1]
                if j % 2:
                    f.act(h_[:, j, 0:tw], ps[:, 0:tw], AF.Identity, bias=b_, scale=a_)
                else:
                    f.ts("dve", h_[:, j, 0:tw], ps[:, 0:tw], a_, b_, ALU.mult, ALU.add)

            def proj(ps, lhs_fn, m=128):
                for kc in range(8):
                    f.matmul(ps[0:m, 0:tw], lhs_fn(kc), h_[:, kc, 0:tw], start=(kc == 0), stop=(kc == 7))

            for ci, c0 in enumerate((0, 256, 1024, 1152)):
                ps = nxt()
                proj(ps, lambda kc: win[:, kc, c0:c0 + 128])
                if ci < 1:
                    f.act(st_[:, ci, 0:tw], ps[:, 0:tw], AF.Copy, scale=0.125)
                else:
                    f.evac(st_[:, ci, 0:tw], ps[:, 0:tw])
            f.dma("pool", S["QAT"][:, t0:t0 + tw], st_[:, 0, 0:tw])
            f.dma("pool", S["KAT"][:, t0:t0 + tw], st_[:, 1, 0:tw])
            f.dma("pool", S["FLT"][:, t0:t0 + tw].rearrange("(c p) t -> p c t", p=128), st_[:, 2:4, 0:tw])
            for ci, c0 in enumerate((768, 896)):
                ps = nxt()
                proj(ps, lambda kc: win[:, kc, c0:c0 + 128])
                f.evac(pl_[:, ci, 0:tw], ps[:, 0:tw])
            f.dma("pool", S["PLT"][:, t0:t0 + tw].rearrange("(c p) t -> p c t", p=128), pl_[:, :, 0:tw])
            for ci, c0 in enumerate((1280, 1408)):
                ps = nxt()
                proj(ps, lambda kc: win[:, kc, c0:c0 + 128])
                f.evac(qc[:, ci, 0:tw], ps[:, 0:tw])
            ps = nxt()
            proj(ps, lambda kc: win[:, kc, 1536:1664])
            f.evac(kvc[:, 0:tw], ps[:, 0:tw])
            va_ = vast[i % 2]
            for s in range(ns):
                ps = nxt()
                for kc in range(8):
                    f.matmul(ps[:, 0:128], h_[:, kc, s * 128:(s + 1) * 128], win[:, kc, 512:640],
                             start=(kc == 0), stop=(kc == 7))
                f.evac(va_[:, s, :, 0:64], ps[:, 0:128].rearrange("p (h d) -> p h d", h=2))
            f.dma("pool", S["VA"][t0:t0 + tw].rearrange("(s p) h c -> p s h c", p=128), va_[:, 0:ns])
            k_ = kst[i % 2]
            pkr = nxt()
            proj(pkr, lambda kc: wkr[:, kc, :], m=96)
            if c == 0:
                pkrs = nxt()
                proj(pkrs, lambda kc: wkrs[:, kc, :], m=96)
                f.tt("dve", t1[64:96, 0:tw], pkr[64:96, 0:tw], rc[i % 2][64:96, 0:tw], ALU.mult)
                f.tt("dve", t2[64:96, 0:tw], pkrs[64:96, 0:tw], rs[i % 2][64:96, 0:tw], ALU.mult)
                f.tt("pool", k_[64:96, 0, 0:tw], t1[64:96, 0:tw], t2[64:96, 0:tw], ALU.add)
            else:
                f.evac(k_[64:96, 0, 0:tw], pkr[64:96, 0:tw])
            for h in range(1, 2):
                f.copy("pool", k_[64:96, h, 0:tw], k_[64:96, 0, 0:tw])
            for ci in range(2):
                f.act(sq[:, ci, 0:tw], qc[:, ci, 0:tw], AF.Square)
            f.act(sq[:, 2, 0:tw], kvc[:, 0:tw], AF.Square)
            psum_q = nxt()
            for ci in range(2):
                f.matmul(psum_q[:, 0:tw], g.ones[:], sq[:, ci, 0:tw], start=(ci == 0), stop=(ci == 1))
            rstd_from_sum(g, rq[:, 0:tw], psum_q[:, 0:tw], 256)
            psum_k = nxt()
            f.matmul(psum_k[:, 0:tw], g.ones[:], sq[:, 2, 0:tw], start=True, stop=True)
            rstd_from_sum(g, rk[:, 0:tw], psum_k[:, 0:tw], 128)
            for ci in range(2):
                f.stt("dve", qn[:, ci, 0:tw], qc[:, ci, 0:tw], qng[:, ci:ci + 1], rq[:, 0:tw], ALU.mult, ALU.mult)
            f.stt("dve", ckv[:, 0:tw], kvc[:, 0:tw], kvg[:, 0:1], rk[:, 0:tw], ALU.mult, ALU.mult)
            for h in range(2):
                ps = nxt()
                f.matmul(ps[0:64, 0:tw], wuk[:, 64 * h:64 * h + 64], ckv[:, 0:tw], start=True, stop=True)
                f.evac(k_[0:64, h, 0:tw], ps[0:64, 0:tw])
            f.dma("pool", S["KT"][:, :, t0:t0 + tw].rearrange("h r t -> r h t"), k_[:, :, 0:tw])
            vd_ = vdst[i % 2]
            for s in range(ns):
                ps = nxt()
                f.matmul(ps[:, 0:128], ckv[:, s * 128:(s + 1) * 128], wuv[:], start=True, stop=True)
                f.evac(vd_[:, s, :, 0:64], ps[:, 0:128].rearrange("p (h d) -> p h d", h=2))
            f.dma("pool", S["VD"][t0:t0 + tw].rearrange("(s p) h c -> p s h c", p=128), vd_[:, 0:ns])
            q_ = qst[i % 2]
            for h in range(2):
                ps = nxt()
                for ci in range(2):
                    f.matmul(ps[0:96, 0:tw], wuq[:, ci, 96 * h:96 * h + 96], qn[:, ci, 0:tw],
                             start=(ci == 0), stop=(ci == 1))
                f.copy("act", q_[0:64, h, 0:tw], ps[0:64, 0:tw])
                if c == 0:
                    ps2 = nxt()
                    for ci in range(2):
                        f.matmul(ps2[0:96, 0:tw], wuqs[:, ci, h, :], qn[:, ci, 0:tw], start=(ci == 0), stop=(ci == 1))
                    f.tt("dve", t1[64:96, 0:tw], ps[64:96, 0:tw], rc[i % 2][64:96, 0:tw], ALU.mult)
                    f.tt("dve", t2[64:96, 0:tw], ps2[64:96, 0:tw], rs[i % 2][64:96, 0:tw], ALU.mult)
                    f.tt("pool", q_[64:96, h, 0:tw], t1[64:96, 0:tw], t2[64:96, 0:tw], ALU.add)
                else:
                    f.copy("act", q_[64:96, h, 0:tw], ps[64:96, 0:tw])
            f.dma("pool", S["QT"][:, :, t0:t0 + tw].rearrange("h r t -> r h t"), q_[:, :, 0:tw])


def phase_na(g, l, with_ctx):
    f, I, S = g.f, g.I, g.S
    with ExitStack() as es:
        sb = lambda n, s, d=F32: g.sb(es, n, s, d)
        KA = sb("KA", [128, 1, TT], BF16)
        QA = sb("QA", [128, 1, TT], BF16)
        VA = sb("VAs", [128, NTILE, 2, 66], BF16)
        BT = sb("BT", [128, 5, 2, 5, 128], BF16)
        f.dma("sp", KA[:, 0, :], S["KAT"][:, :])
        f.dma("sp", QA[:, 0, :], S["QAT"][:, :])
        for q4 in range(0, NTILE, 11):
            f.dma("sp", VA[:, q4:q4 + 11], S["VA"][q4 * 128:(q4 + 11) * 128].rearrange("(t p) h c -> p t h c", p=128))
        btmp = sb("btmp", [128, 2, 5, 128])
        mtmp = sb("mtmp", [128, 5, 128])
        for v in range(5):
            f.dma("sp", btmp[:], I["nab"][l, v])
            f.dma("sp", mtmp[:], I["namask"][v])
            for h in range(2):
                f.tt("dve", BT[:, v, h], btmp[:, h], mtmp[:], ALU.add)
        PT = [sb("PT%d" % i, [128, 896], BF16) for i in range(3)]
        atok = [sb("atok%d" % i, [128, 128]) for i in range(2)]
        rden = sb("rden", [128, 2])
        aT = [sb("aT%d" % i, [128, 512]) for i in range(2)]
        nblk = 64 + (2 if with_ctx else 0)
        steps = []
        for m in range(nblk):
            isctx = m >= 64
            if not isctx:
                v = {0: 0, 1: 1, 62: 3, 63: 4}.get(m, 2)
                p0 = min(max(m - 2, 0), 59)
                chunks = [(p0 + c) for c in range(5)] + [64, 65]
            else:
                v = 0
                chunks = [64, 65]
            for h in range(2):
                steps.append((m, h, isctx, v, chunks))

        def emit_s(it):
            m, h, isctx, v, chunks = steps[it]
            nch = len(chunks)
            ch, pb = 0, 64 * h
            sA = g.ps[(it * 3) % 6]
            sB = g.ps[(it * 3 + 1) % 6]
            pt_ = PT[it % 3]
            q_ = QA[pb:pb + 64, ch, m * 128:(m + 1) * 128]
            for ci, kt in enumerate(chunks):
                dst = sA[:, ci * 128:(ci + 1) * 128] if ci < 4 else sB[:, (ci - 4) * 128:(ci - 3) * 128]
                bias = (not isctx) and ci < 5
                f.matmul(dst, KA[pb:pb + 64, ch, kt * 128:(kt + 1) * 128], q_, start=True, stop=not bias)
                if bias:
                    f.matmul(dst, g.identb[:], BT[:, v, h, ci, :], start=False, stop=True)
            na = min(nch, 4) * 128
            f.act(pt_[:, 0:na], sA[:, 0:na], AF.Exp)
            if nch > 4:
                nb_ = (nch - 4) * 128
                f.act(pt_[:, 512:512 + nb_], sB[:, 0:nb_], AF.Exp)

        def emit_pv(it):
            m, h, isctx, v, chunks = steps[it]
            nch = len(chunks)
            oP = g.ps[(it * 3 + 2) % 6]
            pt_ = PT[it % 3]
            at_ = atok[m % 2]
            for ci, kt in enumerate(chunks):
                src = pt_[:, ci * 128:(ci + 1) * 128] if ci < 4 else pt_[:, 512 + (ci - 4) * 128:512 + (ci - 3) * 128]
                f.matmul(oP[:, 0:65], src, VA[:, kt, h, 0:65], start=(ci == 0), stop=(ci == nch - 1))
            f.recip(rden[:, h:h + 1], oP[:, 64:65])
            f.ts("dve", at_[:, h * 64:(h + 1) * 64], oP[:, 0:64], rden[:, h:h + 1], None, ALU.mult)
            if h == 1:
                o_ = aT[(m // 4) % 2]
                tp = g.ps[6 + (m % 2)]
                f.transpose(tp[:, 0:128], at_[:, 0:128], g.ident[:])
                f.evac(o_[:, (m % 4) * 128:(m % 4 + 1) * 128], tp[:, 0:128])
                if m % 4 == 3 or m == nblk - 1:
                    tile_i = m // 4
                    w = (m - tile_i * 4 + 1) * 128
                    f.dma("pool", S["MAo"][tile_i // 8][(tile_i % 8) * 128:(tile_i % 8 + 1) * 128, 0:w], o_[:, 0:w])

        n = len(steps)
        for it in range(n + 1):
            if it < n:
                emit_s(it)
            if it >= 1:
                emit_pv(it - 1)
        for k_ in range(3 if with_ctx else 2):
            f.collective(S["MAa"][k_][:, :], S["MAo"][k_][:, :], lambda E, k_=k_: E.collective_compute(
                "AllGather", ALU.bypass, replica_groups=[[0, 1], [2, 3], [4, 5], [6, 7]],
                ins=[S["MAo"][k_].h.ap().opt()], outs=[S["MAa"][k_].h.ap().opt()]))


def phase_pool(g, l, with_ctx):
    f, I, S = g.f, g.I, g.S
    with ExitStack() as es:
        sb = lambda n, s, d=F32: g.sb(es, n, s, d)
        wbd = sb("wbd", [128, 2, 128], BF16)
        psc = sb("psc", [128, 2])
        corr = sb("corr", [128, 2, 16])
        for c in range(2):
            f.dma("pool", wbd[:, c, :], I["pool_wbd"][l, c])
            f.dma("sp", corr[:, c, :], I["poolcorr"][c])
        f.dma("sp", psc[:], I["pool_scT"][l])
        L = NL + 16
        u = sb("u", [128, L])
        a = sb("sa", [128, L])
        b = sb("sbb", [128, L])
        pooled = sb("pooled", [128, NL], BF16)
        yst = [sb("yst%d" % i, [128, 512]) for i in range(2)]
        seqs = [(0, NL)] + ([(NL, NC_)] if with_ctx else [])
        k = 0
        for (t0, n) in seqs:
            for c in range(2):
                Ln = n + 16
                f.memset("pool", u[:, 0:8], 0.0)
                f.memset("pool", u[:, 8 + n:16 + n], 0.0)
                f.dma("sp", u[:, 8:8 + n], S["PLT"][c * 128:(c + 1) * 128, t0:t0 + n])
                f.tt("dve", a[:, 0:Ln - 1], u[:, 0:Ln - 1], u[:, 1:Ln], ALU.add)
                if c == 0:
                    f.ts("dve", b[0:64, 8:8 + n], a[0:64, 7:7 + n], 0.5, None, ALU.mult)
                    f.stt("dve", b[64:128, 8:8 + n], a[64:128, 6:6 + n], 1.0, a[64:128, 8:8 + n], ALU.mult, ALU.add)
                    f.ts("dve", b[64:128, 8:8 + n], b[64:128, 8:8 + n], 0.25, None, ALU.mult)
                else:
                    f.tt("dve", b[:, 0:Ln - 3], a[:, 0:Ln - 3], a[:, 2:Ln - 1], ALU.add)
                    f.tt("dve", a[:, 0:Ln - 7], b[:, 0:Ln - 7], b[:, 4:Ln - 3], ALU.add)
                    f.stt("dve", b[64:128, 8:8 + n], a[64:128, 0:n], 1.0, a[64:128, 8:8 + n], ALU.mult, ALU.add)
                    f.ts("dve", b[64:128, 8:8 + n], b[64:128, 8:8 + n], 1.0 / 16, None, ALU.mult)
                    f.ts("dve", b[0:64, 8:8 + n], a[0:64, 4:4 + n], 0.125, None, ALU.mult)
                f.tt("dve", b[:, 8:16], b[:, 8:16], corr[:, c, 0:8], ALU.mult)
                f.tt("dve", b[:, n:8 + n], b[:, n:8 + n], corr[:, c, 8:16], ALU.mult)
                f.tt("dve", pooled[:, 0:n], b[:, 8:8 + n], u[:, 8:8 + n], ALU.subtract)
                for q0 in range(0, n, 512):
                    w = min(512, n - q0)
                    ps = g.ps[k % 4]
                    y_ = yst[k % 2]
                    k += 1
                    f.matmul(ps[:, 0:w], wbd[:, c, :], pooled[:, q0:q0 + w], start=True, stop=True)
                    f.ts("dve", y_[:, 0:w], ps[:, 0:w], psc[:, c:c + 1], None, ALU.mult)
                    f.dma("pool", S["MIXT"][256 + c * 128:256 + (c + 1) * 128, t0 + q0:t0 + q0 + w], y_[:, 0:w])


def phase_fft(g, l, with_ctx):
    f, I, S = g.f, g.I, g.S
    sc_l = 1.0 / math.sqrt(NL * 64.0)
    sc_c = 1.0 / math.sqrt(NC_ * 64.0)
    with ExitStack() as es:
        sb = lambda n, s, d=F32: g.sb(es, n, s, d)
        FL = sb("FL", [128, 2, TT], BF16)
        for c in range(2):
            f.dma("sp", FL[:, c, :], S["FLT"][c * 128:(c + 1) * 128, :])
        fno = sb("fno", [128, 2, 256])
        bdc = sb("bdc", [128, 2, 128])
        bds = sb("bds", [128, 2, 128])
        f.dma("sp", fno[:], I["fno_w"][l].rearrange("(c p) n -> p c n", p=128))
        f.dma("sp", bdc[:], I["bdc64"][:].rearrange("c p n -> p c n"))
        f.dma("sp", bds[:], I["bds64"][:].rearrange("c p n -> p c n"))
        Wc = sb("Wc", [128, 2, 256], BF16)
        Wsn = sb("Wsn", [128, 2, 256], BF16)
        Wcc = sb("Wcc", [128, 2, 256], BF16)
        Wsnc = sb("Wsnc", [128, 2, 256], BF16)
        for c in range(2):
            ps = g.ps[c]
            f.matmul(ps[:, 0:256], bdc[:, c, :], fno[:, c, :], start=True, stop=True)
            f.act(Wc[:, c, :], ps[:, 0:256], AF.Copy, scale=sc_l)
            f.act(Wcc[:, c, :], ps[:, 0:256], AF.Copy, scale=sc_c)
            ps = g.ps[2 + c]
            f.matmul(ps[:, 0:256], bds[:, c, :], fno[:, c, :], start=True, stop=True)
            f.act(Wsn[:, c, :], ps[:, 0:256], AF.Copy, scale=-sc_l)
            f.act(Wsnc[:, c, :], ps[:, 0:256], AF.Copy, scale=-sc_c)
        c128 = sb("c128", [128, 128], BF16)
        s128 = sb("s128", [128, 128], BF16)
        s128n = sb("s128n", [128, 128], BF16)
        f.dma("pool", c128[:], I["c128"][:, :])
        f.dma("pool", s128[:], I["s128"][:, :])
        f.ts("dve", s128n[:], s128[:], -1.0, None, ALU.mult)
        with ExitStack() as es1:
            Ar = g.sb(es1, "Ar", [128, 64, 256], BF16)
            Ai = g.sb(es1, "Ai", [128, 64, 256], BF16)
            bst = [g.sb(es1, "bst%d" % i, [128, 2, 2, 256], BF16) for i in range(4)]
            k = 0
            for n2 in range(0, 64, 2):
                for (W_, A_) in ((Wc, Ar), (Wsn, Ai)):
                    ps = g.ps[k % 4]
                    k += 1
                    for j in range(2):
                        for c in range(2):
                            f.matmul(ps[:, j * 256:(j + 1) * 256], FL[:, c, n2 + j:NL:64], W_[:, c, :],
                                     start=(c == 0), stop=(c == 1))
                    f.evac(A_[:, n2:n2 + 2, :], ps[:, :].rearrange("p (j n) -> p j n", j=2))
            for blk in range(32):
                n2 = 2 * blk
                pr = g.ps[4 + (blk % 2) * 2]
                pi = g.ps[5 + (blk % 2) * 2]
                rr = Ar[:, n2:n2 + 2, :]
                ri = Ai[:, n2:n2 + 2, :]
                f.matmul(pr[:, :], c128[:], rr, start=True, stop=False)
                f.matmul(pr[:, :], s128[:], ri, start=False, stop=True)
                f.matmul(pi[:, :], c128[:], ri, start=True, stop=False)
                f.matmul(pi[:, :], s128n[:], rr, start=False, stop=True)
                b_ = bst[blk % 4]
                f.copy("act", b_[:, 0], pr[:, :].rearrange("p (j n) -> p j n", j=2))
                f.copy("dve", b_[:, 1], pi[:, :].rearrange("p (j n) -> p j n", j=2))
                f.dma("pool", S["BR"][:, n2:n2 + 2, :], b_[:, 0])
                f.dma("pool", S["BI"][:, n2:n2 + 2, :], b_[:, 1])
        f.barrier()
        with ExitStack() as es2:
            BrT = g.sb(es2, "BrT", [64, 128, 128], BF16)
            BiT = g.sb(es2, "BiT", [64, 128, 128], BF16)
            c2 = g.sb(es2, "c2", [64, 128, 64], BF16)
            s2 = g.sb(es2, "s2", [64, 128, 64], BF16)
            cT = g.sb(es2, "cT", [128, NL])
            f.dma("pool", c2[:], I["c2"][:])
            f.dma("pool", s2[:], I["s2"][:])
            k = 0
            for half in range(2):
                hs = slice(half * 128, (half + 1) * 128)
                for q in range(4):
                    f.dma("sp", BrT[:, q * 32:(q + 1) * 32, :], S["BR"][q * 32:(q + 1) * 32, :, hs].rearrange("k n c -> n k c"))
                    f.dma("sp", BiT[:, q * 32:(q + 1) * 32, :], S["BI"][q * 32:(q + 1) * 32, :, hs].rearrange("k n c -> n k c"))
                for k1_0 in range(0, 128, 8):
                    ps = g.ps[k % 4]
                    k += 1
                    for j in range(8):
                        k1 = k1_0 + j
                        f.matmul(ps[:, j * 64:(j + 1) * 64], BrT[:, k1, :], c2[:, k1, :], start=True, stop=False)
                        f.matmul(ps[:, j * 64:(j + 1) * 64], BiT[:, k1, :], s2[:, k1, :], start=False, stop=True)
                    dst = cT[:, :].rearrange("p (k2 k1) -> p k2 k1", k1=128)[:, :, k1_0:k1_0 + 8]
                    f.evac(dst, ps[:, :].rearrange("p (j k2) -> p k2 j", j=8))
                f.dma("pool", S["MIXT"][512 + half * 128:512 + (half + 1) * 128, 0:NL], cT[:, :])
        if with_ctx:
            f.barrier()
            with ExitStack() as es3:
                c256 = g.sb(es3, "c256", [128, 2, 256], BF16)
                s256 = g.sb(es3, "s256", [128, 2, 256], BF16)
                f.dma("pool", c256[:], I["c256"][:].rearrange("(c p) n -> p c n", p=128))
                f.dma("pool", s256[:], I["s256"][:].rearrange("(c p) n -> p c n", p=128))
                Acr = g.sb(es3, "Acr", [128, 2, 256], BF16)
                Aci = g.sb(es3, "Aci", [128, 2, 256], BF16)
                ccT = g.sb(es3, "ccT", [128, 2, 256])
                for nt in range(2):
                    for wi, (W_, A_) in enumerate(((Wcc, Acr), (Wsnc, Aci))):
                        ps = g.ps[nt * 2 + wi]
                        for c in range(2):
                            f.matmul(ps[:, 0:256], FL[:, c, NL + nt * 128:NL + (nt + 1) * 128], W_[:, c, :],
                                     start=(c == 0), stop=(c == 1))
                        f.evac(A_[:, nt, :], ps[:, 0:256])
                for half in range(2):
                    ps = g.ps[4 + half]
                    i = 0
                    for nt in range(2):
                        for (A_, M_) in ((Acr, c256), (Aci, s256)):
                            f.matmul(ps[:, 0:256], A_[:, nt, half * 128:(half + 1) * 128], M_[:, nt, :],
                                     start=(i == 0), stop=(i == 3))
                            i += 1
                    f.evac(ccT[:, half, :], ps[:, 0:256])
                    f.dma("pool", S["MIXT"][512 + half * 128:512 + (half + 1) * 128, NL:TT], ccT[:, half, :])


def phase_mla(g, l, with_ctx):
    f, I, S = g.f, g.I, g.S
    scale = 96.0 ** -0.5
    with ExitStack() as es:
        sb = lambda n, s, d=F32: g.sb(es, n, s, d)
        KT = [sb("KTs%d" % i, [96, TT], BF16) for i in range(2)]
        VD = [sb("VDs%d" % i, [128, NTILE, 128], BF16) for i in range(2)]
        qs = [sb("qs%d" % i, [96, 512], BF16) for i in range(2)]
        P = [sb("P%d" % i, [128, 512], BF16) for i in range(4)]
        osb = sb("osb", [128, 512])
        rdn = sb("rdn", [64, 512])
        dT = [sb("dT%d" % i, [64, 512]) for i in range(2)]
        sel = sb("sel", [128, 64])
        f.dma("sp", sel[:], I["sel"][:, :])
        qtiles = [(i * 512, 512, False) for i in range(16)] + ([(NL, 256, True)] if with_ctx else [])

        def loadkv(h):
            f.dma("sp", KT[h % 2][:, :], S["KT"][h])
            for q in range(0, NTILE, 22):
                f.dma("sp", VD[h % 2][:, q:q + 22, :],
                      S["VD"][q * 128:(q + 22) * 128, h, :].rearrange("(t p) c -> p t c", p=128))

        loadkv(0)
        it = 0
        for h in range(2):
            if h + 1 < 2:
                loadkv(h + 1)
            K_ = KT[h % 2]
            V_ = VD[h % 2]
            f.dma("sp", qs[0][:, 0:512], S["QT"][h, :, 0:512])
            for qi, (t0, tw, isctx) in enumerate(qtiles):
                if qi + 1 < len(qtiles):
                    n0, nw, _ = qtiles[qi + 1]
                    f.dma("sp", qs[(qi + 1) % 2][:, 0:nw], S["QT"][h, :, n0:n0 + nw])
                q_ = qs[qi % 2]
                kts = [64, 65] if isctx else list(range(NTILE))
                oP = g.ps[6 + (qi % 2)]
                n = len(kts)
                LA = 2
                base = it
                for i in range(n + LA):
                    if i < n:
                        kt = kts[i]
                        sP = g.ps[(base + i) % 4]
                        f.matmul(sP[:, 0:tw], K_[:, kt * 128:(kt + 1) * 128], q_[:, 0:tw], start=True, stop=True)
                        f.act(P[(base + i) % 4][:, 0:tw], sP[:, 0:tw], AF.Exp, scale=scale)
                    j = i - LA
                    if j >= 0:
                        f.matmul(oP[:, 0:tw], V_[:, kts[j], :], P[(base + j) % 4][:, 0:tw], start=(j == 0), stop=(j == n - 1))
                it += n
                f.copy("dve", osb[:, 0:tw], oP[:, 0:tw])
                dP = g.ps[4 + (qi % 2)]
                f.matmul(dP[0:64, 0:tw], sel[:], osb[:, 0:tw], start=True, stop=True)
                f.recip(rdn[:, 0:tw], dP[0:64, 0:tw])
                d_ = dT[qi % 2]
                f.tt("dve", d_[:, 0:tw], osb[0:64, 0:tw], rdn[:, 0:tw], ALU.mult)
                f.dma("pool", S["MDo"][qi // 8][(qi % 8) * 128 + h * 64:(qi % 8) * 128 + (h + 1) * 64, 0:tw], d_[:, 0:tw])
        nck = 3 if with_ctx else 2
        for k_ in range(nck):
            f.collective(S["MDa"][k_][:, :], S["MDo"][k_][:, :], lambda E, k_=k_: E.collective_compute(
                "AllGather", ALU.bypass, replica_groups=[[0, 1], [2, 3], [4, 5], [6, 7]],
                ins=[S["MDo"][k_].h.ap().opt()], outs=[S["MDa"][k_].h.ap().opt()]))


def phase_merge(g, l, X, with_ctx):
    f, I, S = g.f, g.I, g.S
    with ExitStack() as es:
        sb = lambda n, s, d=F32: g.sb(es, n, s, d)
        ncv = 2 if with_ctx else 1
        Wo = [sb("Wo%d" % c, [128, 8, 1024], BF16) for c in range(ncv)]
        A2b = [sb("A2b%d" % c, [128, 1024]) for c in range(ncv)]
        B2b = [sb("B2b%d" % c, [128, 1024]) for c in range(ncv)]
        rw = sb("rw", [128, 8, 16])
        gng = sb("gng", [128, 8])
        f.dma("sp", rw[:], I["router_w"][l].rearrange("(j p) n -> p j n", p=128))
        f.dma("sp", gng[:], I["gng"][l])
        with ExitStack() as es0:
            gtb = [g.sb(es0, "gtb%d" % c, [128, 1024]) for c in range(ncv)]
            tmpd = g.sb(es0, "tmpd", [128, 128])
            for c in range(ncv):
                bcast_vec(g, gtb[c], g.mods[:, l, 2, :, c], tmpd, g.ps[6], g.ps[7])
                bcast_vec(g, A2b[c], g.A2[:, l, :, c], tmpd, g.ps[6], g.ps[7])
                bcast_vec(g, B2b[c], g.mods[:, l, 3, :, c], tmpd, g.ps[6], g.ps[7])
            wst = [g.sb(es0, "wst%d" % i, [128, 1024]) for i in range(2)]
            for kc in range(8):
                w_ = wst[kc % 2]
                f.dma("sp", w_[:], I["w_out"][l, kc * 128:(kc + 1) * 128, :])
                for c in range(ncv):
                    f.tt("dve" if c == 0 else "pool", Wo[c][:, kc, :], w_[:], gtb[c][:], ALU.mult)
            f.barrier()
        mx = [sb("mx%d" % i, [128, 8, 512]) for i in range(2)]
        x1 = [sb("x1%d" % i, [128, 4, 1024]) for i in range(3)]
        sqt = sb("sqt", [128, 2, 512])
        rg = sb("rg", [128, 512])
        catT = sb("catT", [128, 8, 512], BF16)
        xn2 = sb("xn2", [128, 4, 1024])
        h2row = sb("h2row", [128, 4, RW], BF16)
        f.memset("pool", h2row[:], 0.0)
        for s_ in range(4):
            f.copy("pool", h2row[:, s_, 1025:1026], g.pidxb[:])
        h2T = sb("h2T", [128, 8, 512])
        tmp = sb("tmp", [128, 1024])
        junk = sb("junk", [128, 1024], BF16)
        ss = sb("ss2", [128, 4])
        rstd = sb("rstd2", [128, 4])
        ex = sb("ex", [128, 4, 16])
        se = sb("se", [128, 4])
        tl = [t for t in tiles512() if (with_ctx or t[2] == 0)]

        def load(i):
            t0, tw, c = tl[i]
            ns = tw // 128
            f.dma("sp", mx[i % 2][:, 2:6, 0:tw], S["MIXT"][256:768, t0:t0 + tw].rearrange("(j p) t -> p j t", p=128))
            ck, n_ = i // 8, (8, 8, 1)[i // 8]
            for rr in range(2):
                r0 = (rr * n_ + (i % 8)) * 128
                f.dma("sp", mx[i % 2][:, rr, 0:tw], S["MAa"][ck][r0:r0 + 128, 0:tw])
                f.dma("sp", mx[i % 2][:, 6 + rr, 0:tw], S["MDa"][ck][r0:r0 + 128, 0:tw])
            f.dma("sp", x1[i % 3][:, 0:ns, :], X[t0:t0 + tw, :].rearrange("(s p) d -> p s d", p=128))

        def stage_a(i):
            t0, tw, c = tl[i]
            ns = tw // 128
            m_ = mx[i % 2]
            x_ = x1[i % 3]
            for grp in range(4):
                for ci in range(2):
                    f.act(sqt[:, ci, 0:tw], m_[:, 2 * grp + ci, 0:tw], AF.Square)
                ps = g.ps[2 + grp % 2]
                for ci in range(2):
                    f.matmul(ps[:, 0:tw], g.ones[:], sqt[:, ci, 0:tw], start=(ci == 0), stop=(ci == 1))
                rstd_from_sum(g, rg[:, 0:tw], ps[:, 0:tw], 256)
                for ci in range(2):
                    j = 2 * grp + ci
                    f.stt("dve", catT[:, j, 0:tw], m_[:, j, 0:tw], gng[:, j:j + 1], rg[:, 0:tw], ALU.mult, ALU.mult)
            for s in range(ns):
                for half in range(2):
                    ps = g.ps[4 + half]
                    for kc in range(8):
                        f.matmul(ps[:, :], catT[:, kc, s * 128:(s + 1) * 128], Wo[c][:, kc, half * 512:(half + 1) * 512],
                                 start=(kc == 0), stop=(kc == 7))
                    f.tt("dve", x_[:, s, half * 512:(half + 1) * 512], ps[:, :], x_[:, s, half * 512:(half + 1) * 512], ALU.add)

        def stage_b(i):
            t0, tw, c = tl[i]
            ns = tw // 128
            x_ = x1[i % 3]
            for s in range(ns):
                f.act(junk[:], x_[:, s, :], AF.Square, accum_out=ss[:, s:s + 1])
            rstd_from_sum(g, rstd[:, 0:ns], ss[:, 0:ns], 1024)
            for s in range(ns):
                f.act(xn2[:, s, :], x_[:, s, :], AF.Identity, scale=rstd[:, s:s + 1])
            f.act(x_[:, 0:ns, :], x_[:, 0:ns, :], AF.Identity, scale=g.rflag[:, 0:1])
            f.dma("pool", g.X1[t0:t0 + tw, :].rearrange("(s p) d -> p s d", p=128), x_[:, 0:ns, :])
            for s in range(ns):
                f.tt("pool", tmp[:], xn2[:, s, :], A2b[c][:], ALU.mult)
                f.tt("pool", h2row[:, s, 0:1024], tmp[:], B2b[c][:], ALU.add)
                f.memset("pool", h2row[:, s, 1024:1025], float(t0 // 128 + s))
            f.dma("pool", S["H2"][t0:t0 + tw, :].rearrange("(s p) d -> p s d", p=128), h2row[:, 0:ns, :])
            for j in range(8):
                ps = g.ps[j % 2]
                for s in range(ns):
                    f.transpose(ps[:, s * 128:(s + 1) * 128], xn2[:, s, j * 128:(j + 1) * 128], g.ident[:])
                a_ = g.A2[:, l, j, c:c + 1]
                b_ = g.mods[:, l, 3, j, c:c + 1]
                if j % 2:
                    f.act(h2T[:, j, 0:tw], ps[:, 0:tw], AF.Identity, bias=b_, scale=a_)
                else:
                    f.ts("dve", h2T[:, j, 0:tw], ps[:, 0:tw], a_, b_, ALU.mult, ALU.add)
            psl = g.ps[6]
            for s in range(ns):
                for kc in range(8):
                    f.matmul(psl[:, s * 16:(s + 1) * 16], h2T[:, kc, s * 128:(s + 1) * 128], rw[:, kc, :],
                             start=(kc == 0), stop=(kc == 7))
            for s in range(ns):
                f.act(ex[:, s, :], psl[:, s * 16:(s + 1) * 16], AF.Exp, accum_out=se[:, s:s + 1])
            f.recip(se[:, 0:ns], se[:, 0:ns])
            for s in range(ns):
                f.ts("dve", g.AFF[:, t0 // 128 + s, :], ex[:, s, :], se[:, s:s + 1], None, ALU.mult)

        load(0)
        n_t = len(tl)
        for i in range(n_t + 1):
            if i < n_t:
                if i + 1 < n_t:
                    load(i + 1)
                stage_a(i)
            if i >= 1:
                stage_b(i - 1)
        f.dma("sp", S["AFFD"][0:TT, :].rearrange("(t p) e -> p t e", p=128), g.AFF[:])


def route_sets(with_ctx):
    return [(0, 64, CAP_L, 0)] + ([(64, 2, CAP_C, CAP_L)] if with_ctx else [])


def phase_route(g, l, with_ctx, niter=30):
    f, I, S = g.f, g.I, g.S
    with ExitStack() as es:
        sb = lambda n, s, d=F32: g.sb(es, n, s, d)
        lo = sb("lo", [128, 16]); hi = sb("hi", [128, 16]); mid = sb("mid", [128, 16])
        d1 = sb("d1", [128, 16]); d2 = sb("d2", [128, 16]); ge = sb("ge", [128, 16]); part = sb("part", [128, 16])
        cmp = sb("cmp", [128, 16, 64])
        trib = sb("trib", [128, 128], BF16)
        f.dma("pool", trib[:], I["tri"][:, :])
        maskf = sb("maskf", [128, 64, 16])
        maskb = sb("maskb", [128, 64, 16], BF16)
        pos = sb("pos", [128, 64, 16])
        cnt = sb("cnt", [128, 64, 16])
        cum = sb("cum", [128, 16, 64])
        ones64 = sb("ones64", [128, 64])
        f.memset("dve", ones64[:], 1.0)
        eoffi = sb("eoffi", [128, 16], I32)
        eoff = sb("eoff", [128, 16])
        f.op("pool", lambda E: E.iota(eoffi.h[:], pattern=[[CAP_L + CAP_C, 16]], base=0, channel_multiplier=0), [eoffi[:]], [])
        f.copy("dve", eoff[:], eoffi[:])
        for (t0, nt, cap, slot0) in route_sets(with_ctx):
            AFv = g.AFF[:, t0:t0 + nt, :]
            AFe = AFv.rearrange("p t e -> p e t")
            f.memset("dve", lo[:], 0.0)
            f.memset("dve", hi[:], 2.0)
            for it in range(niter):
                f.tt("dve", mid[:], lo[:], hi[:], ALU.add)
                f.ts("dve", mid[:], mid[:], 0.5, None, ALU.mult)
                f.tt("dve", cmp[:, :, 0:nt], AFe, mid[:, :].unsqueeze(2).to_broadcast([128, 16, nt]), ALU.is_ge)
                f.reduce("dve", part[:], cmp[:, :, 0:nt], AX.X, ALU.add)
                ps = g.ps[it % 2]
                f.matmul(ps[:, 0:16], g.ones[:], part[:], start=True, stop=True)
                f.ts("dve", ge[:], ps[:, 0:16], float(cap) - 0.5, None, ALU.is_ge)
                f.tt("dve", d1[:], mid[:], lo[:], ALU.subtract)
                f.tt("dve", d1[:], d1[:], ge[:], ALU.mult)
                f.tt("dve", d2[:], hi[:], mid[:], ALU.subtract)
                f.tt("dve", d2[:], d2[:], ge[:], ALU.mult)
                f.tt("dve", lo[:], lo[:], d1[:], ALU.add)
                f.tt("dve", hi[:], mid[:], d2[:], ALU.add)
            f.tt("dve", maskf[:, 0:nt, :], AFv, lo[:, :].unsqueeze(1).to_broadcast([128, nt, 16]), ALU.is_ge)
            f.copy("dve", maskb[:, 0:nt, :], maskf[:, 0:nt, :])
            n = nt * 16
            mflat = maskb[:, 0:nt, :].rearrange("p t e -> p (t e)")
            pflat = pos[:, 0:nt, :].rearrange("p t e -> p (t e)")
            cflat = cnt[:, 0:nt, :].rearrange("p t e -> p (t e)")
            for q0 in range(0, n, 512):
                w = min(512, n - q0)
                pa = g.ps[2]
                pb = g.ps[3]
                f.matmul(pa[:, 0:w], trib[:], mflat[:, q0:q0 + w], start=True, stop=True)
                f.matmul(pb[:, 0:w], g.onesb[:], mflat[:, q0:q0 + w], start=True, stop=True)
                f.copy("dve", pflat[:, q0:q0 + w], pa[:, 0:w])
                f.copy("act", cflat[:, q0:q0 + w], pb[:, 0:w])
            for e in range(16):
                f.op("dve", lambda E, e=e: E.tensor_tensor_scan(
                    cum.h[:, e, 0:nt], ones64.h[:, 0:nt], cnt.h[:, 0:nt, e], 0.0, ALU.mult, ALU.add),
                    [cum[:, e, 0:nt]], [ones64[:], cnt[:]])
            f.tt("dve", pos[:, 0:nt, :], pos[:, 0:nt, :], cum[:, :, 0:nt].rearrange("p e t -> p t e"), ALU.add)
            f.tt("dve", pos[:, 0:nt, :], pos[:, 0:nt, :], cnt[:, 0:nt, :], ALU.subtract)
            f.ts("dve", cnt[:, 0:nt, :], pos[:, 0:nt, :], float(cap) - 0.5, None, ALU.is_lt)
            f.tt("dve", maskf[:, 0:nt, :], maskf[:, 0:nt, :], cnt[:, 0:nt, :], ALU.mult)
            f.tt("dve", pos[:, 0:nt, :], pos[:, 0:nt, :], eoff[:, :].unsqueeze(1).to_broadcast([128, nt, 16]), ALU.add)
            f.stt("dve", pos[:, 0:nt, :], pos[:, 0:nt, :], float(slot0) - BIG, maskf[:, 0:nt, :], ALU.add, ALU.mult)
            f.ts("dve", pos[:, 0:nt, :], pos[:, 0:nt, :], BIG, None, ALU.add)
            f.copy("dve", g.IDX[:, t0:t0 + nt, :], pos[:, 0:nt, :])
        f.dma("sp", S["DBG"][:, 2048:2048 + NTILE * NEXP].bitcast(I32), g.IDX[:].rearrange("p t e -> p (t e)"))


def dispatch_tiles(g, with_ctx):
    out = []
    for (t0, nt, cap, slot0) in route_sets(with_ctx):
        out += list(range(t0, t0 + nt))
    return out


def dispatch_slice(g, hrow, kctr, experts, tiles):
    f, S = g.f, g.S
    XS = S["XS"]
    for t in tiles:
        h_ = hrow[kctr[0] % len(hrow)]
        kctr[0] += 1
        f.dma("sp", h_[:], S["H2"][t * 128:(t + 1) * 128, :])
        for e in experts:
            f.dma("pool", XS[:, :], h_[:], fn=lambda E, e=e, t=t, h_=h_: E.indirect_dma_start(
                out=XS.h.ap(), out_offset=bass.IndirectOffsetOnAxis(ap=g.IDX.h[:, t, e:e + 1], axis=0),
                in_=h_.h[:], in_offset=None, bounds_check=g.breg, oob_is_err=False))


EGRP = 4


def phase_dispatch(g, l, with_ctx):
    with ExitStack() as es:
        hrow = [g.sb(es, "hrow%d" % i, [128, RW], BF16) for i in range(3)]
        dispatch_slice(g, hrow, [0], list(range(EGRP)), dispatch_tiles(g, with_ctx))


def phase_experts(g, l, with_ctx):
    f, I, S = g.f, g.I, g.S
    X1e = [T(g.X1.h, "X1acc%d" % e, loose=True) for e in range(NEL)]
    with ExitStack() as es:
        sb = lambda n, s, d=F32: g.sb(es, n, s, d)
        ncv = 2 if with_ctx else 1
        gtb = [sb("gt2b%d" % c, [128, 1024]) for c in range(ncv)]
        tmpd = sb("tmpd", [128, 128])
        for c in range(ncv):
            bcast_vec(g, gtb[c], g.mods[:, l, 5, :, c], tmpd, g.ps[6], g.ps[7])
        W = [[sb("W%s%d" % (nm, i), [128, 8, 1024], BF16) for nm in ("g", "u", "d")] for i in range(2)]
        xs = sb("xs", [128, 8, RW], BF16)
        xsc = sb("xsc", [32, RW], BF16)
        xsT = sb("xsT", [128, 8, 1056], BF16)
        hidT = sb("hidT", [128, 8, 1056], BF16)
        sg = [sb("sg%d" % i, [128, 512]) for i in range(2)]
        yrow = [sb("yrow%d" % i, [128, 1024]) for i in range(3)]
        for i in range(3):
            f.memset("dve", yrow[i][:], 0.0)
        ranges = [(0, 512), (512, 512)] + ([(1024, 32)] if with_ctx else [])
        hrow = [sb("hrowx%d" % i, [128, RW], BF16) for i in range(2)]
        hk = [0]
        dtiles = dispatch_tiles(g, with_ctx)

        def loadxs(e):
            f.dma("sp", xs[:], S["XS"][e * 1056:e * 1056 + 1024, :].rearrange("(st p) d -> p st d", p=128))
            if with_ctx:
                f.dma("sp", xsc[:], S["XS"][e * 1056 + 1024:e * 1056 + 1056, :])

        wstg = [sb("wstg%d" % i, [128, 512]) for i in range(8)]

        import os
        XP = os.environ.get("XP", "")

        def w_issue(e, j0, j1):
            if "now" in XP and e > 0:
                return
            for j in range(j0, min(j1, 48)):
                wi, kc, hf = j // 16, (j % 16) // 2, j % 2
                nm = ("wg", "wu", "wd")[wi]
                f.dma("sp", wstg[j % 8][:], I[nm][l, e, kc * 128:(kc + 1) * 128, hf * 512:(hf + 1) * 512])

        def w_cast(e, j0, j1):
            if "now" in XP and e > 0:
                return
            for j in range(j0, min(j1, 48)):
                wi, kc, hf = j // 16, (j % 16) // 2, j % 2
                f.copy("act" if j % 2 else "dve", W[e % 2][wi][:, kc, hf * 512:(hf + 1) * 512], wstg[j % 8][:])

        def loadw(e):
            w_issue(e, 0, 6)
            for i in range(8):
                w_cast(e, 6 * i, 6 * i + 6)
                w_issue(e, 6 * i + 6, 6 * i + 12)

        tokf = [sb("tokf%d" % i, [128, 9]) for i in range(2)]
        toki = [sb("toki%d" % i, [128, 9], I32) for i in range(2)]
        affr = [sb("affr%d" % i, [128, 9, NEXP]) for i in range(2)]
        nst = 9 if with_ctx else 8

        def tok_and_gates(e):
            tf, ti_, af = tokf[e % 2], toki[e % 2], affr[e % 2]
            f.stt("dve", tf[:, 0:8], xs[:, :, 1024], 128.0, xs[:, :, 1025], ALU.mult, ALU.add)
            f.copy("dve", ti_[:, 0:8], tf[:, 0:8])
            if with_ctx:
                f.copy("dve", ti_[:, 8:9], g.dumi[:])
                f.stt("dve", tf[0:32, 8:9], xsc[0:32, 1024:1025], 128.0, xsc[0:32, 1025:1026], ALU.mult, ALU.add)
                f.copy("dve", ti_[0:32, 8:9], tf[0:32, 8:9])
            for st in range(nst):
                f.dma("pool", af[:, st, :], S["AFFD"][:, :], deps=[ti_[:]], fn=lambda E, st=st: E.indirect_dma_start(
                    out=af.h[:, st, :], out_offset=None, in_=S["AFFD"].h.ap(),
                    in_offset=bass.IndirectOffsetOnAxis(ap=ti_.h[:, st:st + 1], axis=0),
                    bounds_check=g.breg2, oob_is_err=False))

        loadw(0)
        loadxs(0)
        tok_and_gates(0)
        k = 0
        yk = 0
        for e in range(NEL):
            Wg, Wu, Wd = W[e % 2]
            ti_, af = toki[e % 2], affr[e % 2]
            pc = g.ps[7][:, :].bitcast(BF16)
            for kc in range(8):
                pb = g.ps[kc % 2][:, :].bitcast(BF16)
                for st in range(8):
                    f.transpose(pb[:, st * 128:(st + 1) * 128], xs[:, st, kc * 128:(kc + 1) * 128], g.identb[:])
                f.evac(xsT[:, kc, 0:1024], pb[:, 0:1024])
                if with_ctx:
                    f.transpose(pc[:, kc * 32:(kc + 1) * 32], xsc[0:32, kc * 128:(kc + 1) * 128], g.identb[0:32, 0:32])
            if with_ctx:
                f.evac(xsT[:, :, 1024:1056], pc[:, 0:256].rearrange("p (k s) -> p k s", k=8))
            nxt_same_grp = (e + 1 < NEL) and ((e + 1) % EGRP != 0)
            if nxt_same_grp:
                loadxs(e + 1)
            if e + 1 < NEL:
                w_issue(e + 1, 0, 6)
            for fc in range(8):
                if e + 1 < NEL:
                    w_cast(e + 1, 6 * fc, 6 * fc + 6)
                    w_issue(e + 1, 6 * fc + 6, 6 * fc + 12)
                for (s0, sw) in ranges:
                    pg = g.ps[2 + (k % 2) * 2]
                    pu = g.ps[3 + (k % 2) * 2]
                    s_ = sg[k % 2]
                    k += 1
                    for kc in range(8):
                        f.matmul(pg[:, 0:sw], Wg[:, kc, fc * 128:(fc + 1) * 128], xsT[:, kc, s0:s0 + sw],
                                 start=(kc == 0), stop=(kc == 7))
                    for kc in range(8):
                        f.matmul(pu[:, 0:sw], Wu[:, kc, fc * 128:(fc + 1) * 128], xsT[:, kc, s0:s0 + sw],
                                 start=(kc == 0), stop=(kc == 7))
                    f.act(s_[:, 0:sw], pg[:, 0:sw], AF.Silu)
                    f.tt("dve", hidT[:, fc, s0:s0 + sw], s_[:, 0:sw], pu[:, 0:sw], ALU.mult)
            tiles = [(st * 128, 128) for st in range(8)] + ([(1024, 32)] if with_ctx else [])
            for ti, (s0, sw) in enumerate(tiles):
                y_ = yrow[yk % 3]
                yk += 1
                c = 1 if ti == 8 else 0
                for half in range(2):
                    py = g.ps[6 + half]
                    for fc in range(8):
                        f.matmul(py[0:sw, :], hidT[:, fc, s0:s0 + sw], Wd[:, fc, half * 512:(half + 1) * 512],
                                 start=(fc == 0), stop=(fc == 7))
                    f.stt("dve", y_[0:sw, half * 512:(half + 1) * 512], py[0:sw, :], af[0:sw, ti, e:e + 1],
                          gtb[c][0:sw, half * 512:(half + 1) * 512], ALU.mult, ALU.mult)
                f.dma("pool", X1e[e][:, :], y_[:], deps=[ti_[:]] + ([X1e[e - 1][:, :]] if e > 0 else []),
                      fn=lambda E, ti=ti, y_=y_, ti_=ti_: E.indirect_dma_start(
                    out=g.X1.h.ap(), out_offset=bass.IndirectOffsetOnAxis(ap=ti_.h[:, ti:ti + 1], axis=0),
                    in_=y_.h[:], in_offset=None, bounds_check=g.breg2, oob_is_err=True, compute_op=ALU.add))
            if nxt_same_grp:
                tok_and_gates(e + 1)
            grp = e // EGRP
            if (grp + 1) * EGRP < NEL:
                j = e % EGRP
                n_ = len(dtiles)
                sl = dtiles[(j * n_) // EGRP:((j + 1) * n_) // EGRP]
                dispatch_slice(g, hrow, hk, list(range((grp + 1) * EGRP, (grp + 2) * EGRP)), sl)
            if e + 1 < NEL and not nxt_same_grp:
                loadxs(e + 1)
                tok_and_gates(e + 1)


def xg_rows(k):
    return 512 if k < 16 else 256


def phase_exchange(g, l, with_ctx):
    f, I, S = g.f, g.I, g.S
    nck = NXCH if with_ctx else 16
    for k in range(nck):
        n = xg_rows(k)
        f.collective(S["XG"][k][:, :], g.X1[:, :], lambda E, k=k, n=n: E.collective_compute(
            "AllGather", ALU.bypass, replica_groups=[[0, 1], [2, 3], [4, 5], [6, 7]],
            ins=[g.X1.h[k * 512:k * 512 + n, :].opt()], outs=[S["XG"][k].h.ap().opt()]))
    if True:
        return
    with ExitStack() as es:
        a = [g.sb(es, "xa%d" % i, [128, 4, 1024]) for i in range(2)]
        b = [g.sb(es, "xb%d" % i, [128, 4, 1024]) for i in range(2)]
        for k in range(nck):
            n = xg_rows(k)
            ns = n // 128
            a_, b_ = a[k % 2], b[k % 2]
            f.dma("sp", a_[:, 0:ns, :], S["XG"][k][0:n, :].rearrange("(s p) d -> p s d", p=128))
            f.dma("sp", b_[:, 0:ns, :], S["XG"][k][n:2 * n, :].rearrange("(s p) d -> p s d", p=128))
            f.tt("dve" if k % 2 else "pool", a_[:, 0:ns, :], a_[:, 0:ns, :], b_[:, 0:ns, :], ALU.add)
            f.dma("sp", S["X2"][k * 512:k * 512 + n, :].rearrange("(s p) d -> p s d", p=128), a_[:, 0:ns, :])


def phase_final(g):
    f, I, S = g.f, g.I, g.S
    with ExitStack() as es:
        sb = lambda n, s, d=F32: g.sb(es, n, s, d)
        fnb = sb("fnb", [128, 1024])
        f.dma("sp", fnb[:], I["fnorm"][:].partition_broadcast(128))
        xt = [sb("xf%d" % i, [128, 4, 1024]) for i in range(2)]
        xu = [sb("xu%d" % i, [128, 4, 1024]) for i in range(2)]
        junk = sb("junkf", [128, 1024], BF16)
        ss = sb("ssf", [128, 4])
        rstd = sb("rstdf", [128, 4])
        nt = NL // 512

        def load(i):
            f.dma("sp", xt[i % 2][:], S["XG"][i][0:512, :].rearrange("(s p) d -> p s d", p=128))
            f.dma("sp", xu[i % 2][:], S["XG"][i][512:1024, :].rearrange("(s p) d -> p s d", p=128))

        load(0)
        for i in range(nt):
            if i + 1 < nt:
                load(i + 1)
            x_ = xt[i % 2]
            f.tt("dve", x_[:], x_[:], xu[i % 2][:], ALU.add)
            for s in range(4):
                f.act(junk[:], x_[:, s, :], AF.Square, accum_out=ss[:, s:s + 1])
            rstd_from_sum(g, rstd[:], ss[:], 1024)
            for s in range(4):
                f.act(x_[:, s, :], x_[:, s, :], AF.Identity, scale=rstd[:, s:s + 1])
                f.tt("dve" if s % 2 else "pool", x_[:, s, :], x_[:, s, :], fnb[:], ALU.mult)
            f.dma("sp", g.OUT[i * 512:(i + 1) * 512, :].rearrange("(s p) d -> p s d", p=128), x_[:])


_NC_CACHE = {}


def kernel(**inputs):
    inp = {k: np.asarray(v) for k, v in inputs.items()}
    B = inp["x"].shape[0]
    if "nc" not in _NC_CACHE:
        _NC_CACHE["nc"] = build()
    nc = _NC_CACHE["nc"]
    in_maps = [host_prep(inp, c // 2, c % 2) for c in range(2 * B)]
    res = run_bass_kernel_spmd(nc, in_maps, core_ids=list(range(2 * B)))
    out = np.stack([np.asarray(res.results[2 * b]["out"], dtype=np.float32) for b in range(B)], axis=0)
    return out
```
